# Optimizing a Trainium2 kernel written in Bass

```python
import math
import jax, jax.numpy as jnp
from jax import lax
import numpy as np

D_MODEL = 4096
BATCH = 1
SEQ = 8192
DEPTH = 4

CTX_LEN = 256
GRID_W = 64
N_MIXERS = 3
HEAD_DIM = 128
N_HEADS_A = D_MODEL // HEAD_DIM
N_KV_HEADS_A = N_HEADS_A // 4
N_HEADS_C = D_MODEL // (2 * HEAD_DIM)
DIFF_V_DIM = 2 * HEAD_DIM
ROPE_THETA = 10000.0
Q_BLOCK = 128
POOL_WINDOWS = (2, 4, 8, 16)
N_POOL_GROUPS = len(POOL_WINDOWS)
POOL_GROUP_DIM = D_MODEL // N_POOL_GROUPS
N_EXPERT_GROUPS = 4
EXPERTS_PER_GROUP = 4
N_EXPERTS = N_EXPERT_GROUPS * EXPERTS_PER_GROUP
D_EXPERT = 3 * D_MODEL // 32
TOP_K_INNER = 2
N_LAYERS_A = (DEPTH + 2) // 3
N_LAYERS_B = (DEPTH + 1) // 3
N_LAYERS_C = DEPTH // 3
DN_ALPHA = (2 * DEPTH) ** 0.25
DN_BETA = (8 * DEPTH) ** -0.25
LN_EPS = 1e-6
RMS_EPS = 1e-6

kernel_name = 'hybrid_interleaved_gqa_pool_diffattn_hmoe_dit'


def layer_norm(x, g, b):
    xf = x.astype(jnp.float32)
    mu = jnp.mean(xf, axis=-1, keepdims=True)
    var = jnp.mean(jnp.square(xf - mu), axis=-1, keepdims=True)
    return ((xf - mu) * lax.rsqrt(var + LN_EPS) * g + b).astype(x.dtype)


def rms_norm(x, g):
    xf = x.astype(jnp.float32)
    y = xf * lax.rsqrt(jnp.mean(jnp.square(xf), axis=-1, keepdims=True) + RMS_EPS)
    return (y * g).astype(x.dtype)


def axial_rope_tables(n_tokens, dtype):
    rows = n_tokens // GRID_W
    row = jnp.broadcast_to(jnp.arange(rows, dtype=jnp.float32)[:, None], (rows, GRID_W)).reshape(-1)
    col = jnp.broadcast_to(jnp.arange(GRID_W, dtype=jnp.float32)[None, :], (rows, GRID_W)).reshape(-1)
    n_freq = HEAD_DIM // 4
    inv_freq = ROPE_THETA ** (-jnp.arange(n_freq, dtype=jnp.float32) / n_freq)
    ang = jnp.stack([row[:, None] * inv_freq, col[:, None] * inv_freq], axis=1)
    return jnp.cos(ang).astype(dtype), jnp.sin(ang).astype(dtype)


def apply_axial_rope(x, cos, sin):
    b, n, h, hd = x.shape
    xr = x.reshape(b, n, h, 2, 2, hd // 4)
    x1, x2 = xr[..., 0, :], xr[..., 1, :]
    cs, sn = cos[None, :, None], sin[None, :, None]
    out = jnp.stack([x1 * cs - x2 * sn, x2 * cs + x1 * sn], axis=-2)
    return out.reshape(b, n, h, hd)


def sweep_query_blocks(attend, qs):
    b, n = qs[0].shape[:2]
    nb = n // Q_BLOCK
    blocks = tuple(jnp.swapaxes(q.reshape((b, nb, Q_BLOCK) + q.shape[2:]), 0, 1) for q in qs)
    out = lax.map(lambda qb: attend(*qb), blocks)
    out = jnp.swapaxes(out, 0, 1)
    return out.reshape((b, n) + out.shape[3:])


def gqa_attend(q, k, v):
    b, nq, h, hd = q.shape
    kvh = k.shape[2]
    qg = q.reshape(b, nq, kvh, h // kvh, hd)
    s = jnp.einsum('bqhgd,bshd->bhgqs', qg, k).astype(jnp.float32) * (1.0 / math.sqrt(hd))
    p = jax.nn.softmax(s, axis=-1).astype(v.dtype)
    o = jnp.einsum('bhgqs,bshd->bqhgd', p, v)
    return o.reshape(b, nq, h, hd)


def gqa_mixer(h, hc, rope_cos, rope_sin, wq, wk, wv, wo, qn_g, kn_g, with_ctx_out):
    b, n, _ = h.shape
    lc = hc.shape[1]
    q = apply_axial_rope(rms_norm((h @ wq).reshape(b, n, N_HEADS_A, HEAD_DIM), qn_g), rope_cos, rope_sin)
    k = apply_axial_rope(rms_norm((h @ wk).reshape(b, n, N_KV_HEADS_A, HEAD_DIM), kn_g), rope_cos, rope_sin)
    v = (h @ wv).reshape(b, n, N_KV_HEADS_A, HEAD_DIM)
    kc = rms_norm((hc @ wk).reshape(b, lc, N_KV_HEADS_A, HEAD_DIM), kn_g)
    vc = (hc @ wv).reshape(b, lc, N_KV_HEADS_A, HEAD_DIM)
    k_all = jnp.concatenate([kc, k], axis=1)
    v_all = jnp.concatenate([vc, v], axis=1)
    o = sweep_query_blocks(lambda qb: gqa_attend(qb, k_all, v_all), (q,))
    y = o.reshape(b, n, -1) @ wo
    if not with_ctx_out:
        return y, None
    qc = rms_norm((hc @ wq).reshape(b, lc, N_HEADS_A, HEAD_DIM), qn_g)
    yc = gqa_attend(qc, kc, vc).reshape(b, lc, -1) @ wo
    return y, yc


def diff_attend(q1, q2, k1, k2, v, lam):
    scale = 1.0 / math.sqrt(q1.shape[-1])
    s1 = jnp.einsum('bqhd,bshd->bhqs', q1, k1).astype(jnp.float32) * scale
    s2 = jnp.einsum('bqhd,bshd->bhqs', q2, k2).astype(jnp.float32) * scale
    p = jax.nn.softmax(s1, axis=-1) - lam * jax.nn.softmax(s2, axis=-1)
    return jnp.einsum('bhqs,bshe->bqhe', p.astype(v.dtype), v)


def diff_mixer(h, hc, rope_cos, rope_sin, wq, wk, wv, wo, lq1, lk1, lq2, lk2, subln_g, lam_init, with_ctx_out):
    b, n, _ = h.shape
    lc = hc.shape[1]
    lam = (jnp.exp(jnp.sum(lq1.astype(jnp.float32) * lk1.astype(jnp.float32)))
           - jnp.exp(jnp.sum(lq2.astype(jnp.float32) * lk2.astype(jnp.float32))) + lam_init)

    def split_qk(z, w, length, rotate):
        t = (z @ w).reshape(b, length, 2 * N_HEADS_C, HEAD_DIM)
        if rotate:
            t = apply_axial_rope(t, rope_cos, rope_sin)
        t = t.reshape(b, length, N_HEADS_C, 2, HEAD_DIM)
        return t[:, :, :, 0], t[:, :, :, 1]

    def finish(o, length):
        o = rms_norm(o, subln_g) * (1.0 - lam_init)
        return o.reshape(b, length, -1) @ wo

    q1, q2 = split_qk(h, wq, n, True)
    k1, k2 = split_qk(h, wk, n, True)
    v = (h @ wv).reshape(b, n, N_HEADS_C, DIFF_V_DIM)
    kc1, kc2 = split_qk(hc, wk, lc, False)
    vc = (hc @ wv).reshape(b, lc, N_HEADS_C, DIFF_V_DIM)
    k1_all = jnp.concatenate([kc1, k1], axis=1)
    k2_all = jnp.concatenate([kc2, k2], axis=1)
    v_all = jnp.concatenate([vc, v], axis=1)
    o = sweep_query_blocks(lambda a, c2: diff_attend(a, c2, k1_all, k2_all, v_all, lam), (q1, q2))
    y = finish(o, n)
    if not with_ctx_out:
        return y, None
    qc1, qc2 = split_qk(hc, wq, lc, False)
    yc = finish(diff_attend(qc1, qc2, kc1, kc2, vc, lam), lc)
    return y, yc


def pool_mixer(h, pool_w, pool_b, pool_scale):
    b, n, d = h.shape
    hg = h.reshape(b, n, N_POOL_GROUPS, POOL_GROUP_DIM)
    csum = jnp.cumsum(hg.astype(jnp.float32), axis=1)
    prefix = jnp.concatenate([jnp.zeros((b, 1, N_POOL_GROUPS, POOL_GROUP_DIM), jnp.float32), csum], axis=1)
    t = jnp.arange(n)[:, None]
    w = jnp.array(POOL_WINDOWS, dtype=jnp.int32)[None, :]
    lo = jnp.clip(t - w // 2, 0, n - 1)
    hi = jnp.clip(t + w // 2 - 1, 0, n - 1)
    gi = jnp.arange(N_POOL_GROUPS)[None, :]
    win_sum = prefix[:, hi + 1, gi] - prefix[:, lo, gi]
    mean = win_sum / (hi - lo + 1).astype(jnp.float32)[None, :, :, None]
    y = (mean - hg.astype(jnp.float32)).astype(h.dtype)
    y = jnp.einsum('bngc,gce->bnge', y, pool_w) + pool_b
    return y.reshape(b, n, d) * pool_scale


def hier_moe(h, rg_w, rg_b, re_w, re_b, w1, w3, w2):
    t = h.shape[0]
    g_logits = (h @ rg_w + rg_b).astype(jnp.float32)
    g_prob = jax.nn.softmax(g_logits, axis=-1)
    _, g_top = lax.top_k(g_logits, 1)
    p_g = jnp.take_along_axis(g_prob, g_top, axis=-1)
    e_logits = (h @ re_w + re_b).astype(jnp.float32).reshape(t, N_EXPERT_GROUPS, EXPERTS_PER_GROUP)
    e_sel = jnp.take_along_axis(e_logits, g_top[:, :, None], axis=1)[:, 0]
    top_v, top_i = lax.top_k(e_sel, TOP_K_INNER)
    top_w = jax.nn.softmax(top_v, axis=-1) * p_g
    expert_idx = g_top * EXPERTS_PER_GROUP + top_i
    gate = jnp.sum(jax.nn.one_hot(expert_idx, N_EXPERTS, dtype=jnp.float32) * top_w[..., None], axis=1)
    a = jnp.einsum('td,edf->tef', h, w1)
    u = jnp.einsum('td,edf->tef', h, w3)
    hid = jax.nn.silu(a) * u * gate[:, :, None].astype(h.dtype)
    return jnp.einsum('tef,efd->td', hid, w2)


def _normal(k, shape, std):
    return jax.random.normal(k, shape, jnp.float32) * std


def setup_inputs(seed: int = 0) -> dict:
    key = jax.random.key(seed)
    ks = jax.random.split(key, 40)
    d = D_MODEL
    ha = N_HEADS_A * HEAD_DIM
    hkv = N_KV_HEADS_A * HEAD_DIM
    hc2 = 2 * N_HEADS_C * HEAD_DIM
    hv = N_HEADS_C * DIFF_V_DIM
    return {
        'x': _normal(ks[0], (BATCH, SEQ, d), 1.0),
        'c': _normal(ks[1], (BATCH, d), 1.0),
        'ctx': _normal(ks[2], (BATCH, CTX_LEN, d), 1.0),
        'c_ctx': _normal(ks[3], (d,), 1.0),
        'ada_w': _normal(ks[4], (DEPTH, d, 6 * d), 0.5 * d ** -0.5),
        'ada_b': _normal(ks[5], (DEPTH, 6 * d), 0.02),
        'ln_g': 1.0 + _normal(ks[6], (DEPTH, 2, d), 0.02),
        'ln_b': _normal(ks[7], (DEPTH, 2, d), 0.02),
        'attn_wq': _normal(ks[8], (N_LAYERS_A, d, ha), d ** -0.5),
        'attn_wk': _normal(ks[9], (N_LAYERS_A, d, hkv), d ** -0.5),
        'attn_wv': _normal(ks[10], (N_LAYERS_A, d, hkv), d ** -0.5),
        'attn_wo': _normal(ks[11], (N_LAYERS_A, ha, d), DN_BETA * ha ** -0.5),
        'attn_qn_g': 1.0 + _normal(ks[12], (N_LAYERS_A, HEAD_DIM), 0.02),
        'attn_kn_g': 1.0 + _normal(ks[13], (N_LAYERS_A, HEAD_DIM), 0.02),
        'pool_w': _normal(ks[14], (N_LAYERS_B, N_POOL_GROUPS, POOL_GROUP_DIM, POOL_GROUP_DIM), DN_BETA * POOL_GROUP_DIM ** -0.5),
        'pool_b': _normal(ks[15], (N_LAYERS_B, N_POOL_GROUPS, POOL_GROUP_DIM), 0.02),
        'pool_scale': 1.0 + _normal(ks[16], (N_LAYERS_B, d), 0.02),
        'diff_wq': _normal(ks[17], (N_LAYERS_C, d, hc2), d ** -0.5),
        'diff_wk': _normal(ks[18], (N_LAYERS_C, d, hc2), d ** -0.5),
        'diff_wv': _normal(ks[19], (N_LAYERS_C, d, hv), d ** -0.5),
        'diff_wo': _normal(ks[20], (N_LAYERS_C, hv, d), DN_BETA * hv ** -0.5),
        'diff_lq1': _normal(ks[21], (N_LAYERS_C, HEAD_DIM), 0.1),
        'diff_lk1': _normal(ks[22], (N_LAYERS_C, HEAD_DIM), 0.1),
        'diff_lq2': _normal(ks[23], (N_LAYERS_C, HEAD_DIM), 0.1),
        'diff_lk2': _normal(ks[24], (N_LAYERS_C, HEAD_DIM), 0.1),
        'diff_subln_g': 1.0 + _normal(ks[25], (N_LAYERS_C, DIFF_V_DIM), 0.02),
        'moe_rg_w': _normal(ks[26], (DEPTH, d, N_EXPERT_GROUPS), d ** -0.5),
        'moe_rg_b': _normal(ks[27], (DEPTH, N_EXPERT_GROUPS), 0.01),
        'moe_re_w': _normal(ks[28], (DEPTH, d, N_EXPERTS), d ** -0.5),
        'moe_re_b': _normal(ks[29], (DEPTH, N_EXPERTS), 0.01),
        'moe_w1': _normal(ks[30], (DEPTH, N_EXPERTS, d, D_EXPERT), d ** -0.5),
        'moe_w3': _normal(ks[31], (DEPTH, N_EXPERTS, d, D_EXPERT), d ** -0.5),
        'moe_w2': _normal(ks[32], (DEPTH, N_EXPERTS, D_EXPERT, d), DN_BETA * D_EXPERT ** -0.5),
    }


def reference(x, c, ctx, c_ctx, ada_w, ada_b, ln_g, ln_b, attn_wq, attn_wk, attn_wv, attn_wo, attn_qn_g, attn_kn_g, pool_w, pool_b, pool_scale, diff_wq, diff_wk, diff_wv, diff_wo, diff_lq1, diff_lk1, diff_lq2, diff_lk2, diff_subln_g, moe_rg_w, moe_rg_b, moe_re_w, moe_re_b, moe_w1, moe_w3, moe_w2):
    b, n, d = x.shape
    lc = ctx.shape[1]
    rope_cos, rope_sin = axial_rope_tables(n, x.dtype)
    silu_c = jax.nn.silu(c)
    silu_cc = jax.nn.silu(c_ctx)
    for i in range(DEPTH):
        last = i == DEPTH - 1
        kind, j = i % N_MIXERS, i // N_MIXERS
        sh1, sc1, g1, sh2, sc2, g2 = jnp.split((silu_c @ ada_w[i] + ada_b[i])[:, None, :], 6, axis=-1)
        csh1, csc1, cg1, csh2, csc2, cg2 = jnp.split(silu_cc @ ada_w[i] + ada_b[i], 6, axis=-1)
        h = x * (1 + sc1) + sh1
        hc = ctx * (1 + csc1) + csh1
        if kind == 0:
            y, yc = gqa_mixer(h, hc, rope_cos, rope_sin, attn_wq[j], attn_wk[j], attn_wv[j], attn_wo[j],
                              attn_qn_g[j], attn_kn_g[j], not last)
        elif kind == 1:
            y = pool_mixer(h, pool_w[j], pool_b[j], pool_scale[j])
            yc = None if last else pool_mixer(hc, pool_w[j], pool_b[j], pool_scale[j])
        else:
            lam_init = 0.8 - 0.6 * math.exp(-0.3 * i)
            y, yc = diff_mixer(h, hc, rope_cos, rope_sin, diff_wq[j], diff_wk[j], diff_wv[j], diff_wo[j],
                               diff_lq1[j], diff_lk1[j], diff_lq2[j], diff_lk2[j], diff_subln_g[j],
                               lam_init, not last)
        x = layer_norm(DN_ALPHA * x + g1 * y, ln_g[i, 0], ln_b[i, 0])
        h = x * (1 + sc2) + sh2
        moe_args = (moe_rg_w[i], moe_rg_b[i], moe_re_w[i], moe_re_b[i], moe_w1[i], moe_w3[i], moe_w2[i])
        if last:
            y2 = hier_moe(h.reshape(b * n, d), *moe_args).reshape(b, n, d)
        else:
            ctx = layer_norm(DN_ALPHA * ctx + cg1 * yc, ln_g[i, 0], ln_b[i, 0])
            hc = ctx * (1 + csc2) + csh2
            tokens = jnp.concatenate([hc, h], axis=1).reshape(b * (lc + n), d)
            out = hier_moe(tokens, *moe_args).reshape(b, lc + n, d)
            ctx = layer_norm(DN_ALPHA * ctx + cg2 * out[:, :lc], ln_g[i, 1], ln_b[i, 1])
            y2 = out[:, lc:]
        x = layer_norm(DN_ALPHA * x + g2 * y2, ln_g[i, 1], ln_b[i, 1])
    return x
```

```python
import math
import contextlib
import numpy as np
import concourse.bass as bass
import concourse.mybir as mybir
from concourse.bass_utils import run_bass_kernel_spmd

F32 = mybir.dt.float32
BF16 = mybir.dt.bfloat16
AF = mybir.ActivationFunctionType
ALU = mybir.AluOpType
AX = mybir.AxisListType

NCORES = 8
D = 4096
NCH = 32
TX = 1024
TC = 32
T = TX + TC
SEQ = 8192
CTX = 256
DEPTH = 4
HD = 128
GRID_W = 64
DN_ALPHA = (2 * DEPTH) ** 0.25
LN_EPS = 1e-6
RMS_EPS = 1e-6
NEXP = 16
DEXP = 384
MCOLS = 6 * D // NCORES
POOL_WINDOWS = (2, 4, 8, 16)

TILES = [(i * 128, 128) for i in range(8)] + [(1024, 32)]
TGROUPS = [(0, 512), (512, 512), (1024, 32)]


class Trk:
    __slots__ = ("name", "writes", "readers", "dsem", "dcount")

    def __init__(self, name):
        self.name = name
        self.writes = {}
        self.readers = {}
        self.dsem = None
        self.dcount = 0


class KB:
    ENG = ("pe", "act", "dve", "pool", "sp")

    def __init__(self):
        self.nc = bass.Bass("TRN2", target_bir_lowering=False)
        nc = self.nc
        self.eng = {"pe": nc.tensor, "act": nc.scalar, "dve": nc.vector, "pool": nc.gpsimd, "sp": nc.sync}
        self.esem = {}
        self.ecount = {}
        for e in self.ENG:
            self.esem[e] = nc.semaphore("sem_" + e).__enter__()
            self.ecount[e] = 0
        self.waited = {}
        self.dtrks = []
        self.dpool = []
        self.dnext = 0
        self.cc = None
        self.root = contextlib.ExitStack()
        self.stack = self.root
        self.ndram = 0

    def sb(self, name, shape, dt):
        self.ndram += 1
        return self.stack.enter_context(self.nc.sbuf_tensor("s%d_%s" % (self.ndram, name), list(shape), dt))

    def ps(self, name, shape, dt=F32):
        return self.stack.enter_context(self.nc.psum_tensor(name, list(shape), dt))

    def dram(self, name, shape, dt, kind="Internal"):
        return self.nc.dram_tensor(name, list(shape), dt, kind=kind).ap()

    @contextlib.contextmanager
    def scope(self):
        prev = self.stack
        st = contextlib.ExitStack()
        self.stack = st
        try:
            yield
        finally:
            self.barrier()
            st.close()
            self.stack = prev

    def _wait(self, e, ev):
        sem, val, trk, src = ev
        if trk is not None:
            val = max(val, trk.dcount)
        if src == e and e == "pe":
            return
        key = (e, id(sem))
        if self.waited.get(key, 0) >= val:
            return
        self.waited[key] = val
        self.eng[e].wait_ge(sem, val)

    def _deps(self, e, reads, writes, part):
        for r in reads:
            for ev in r.writes.values():
                self._wait(e, ev)
        for w in writes:
            if not part:
                for ev in w.writes.values():
                    self._wait(e, ev)
            for ev in w.readers.values():
                self._wait(e, ev)

    def _record(self, ev, reads, writes, part):
        key = id(ev[0])
        for r in reads:
            r.readers[key] = ev
        for w in writes:
            if not part:
                w.writes = {}
                w.readers = {}
            w.writes[key] = ev

    def op(self, e, reads, writes, fn, part=False):
        self._deps(e, reads, writes, part)
        ins = fn()
        self.ecount[e] += 1
        ins.then_inc(self.esem[e], 1)
        ev = (self.esem[e], self.ecount[e], None, e)
        self._record(ev, reads, writes, part)
        return ev

    NDSEM = 32

    def _semtrk(self, semtrk):
        if semtrk.dsem is None:
            if len(self.dpool) < self.NDSEM:
                ds = Trk("dsem%d" % len(self.dpool))
                ds.dsem = self.nc.semaphore("ds%d" % len(self.dpool)).__enter__()
                self.dpool.append(ds)
                self.dtrks.append(ds)
            semtrk.dsem = self.dpool[self.dnext % self.NDSEM]
            self.dnext += 1

    def dma(self, q, out, in_, reads, writes, semtrk, part=True):
        self._deps(q, reads, writes, part)
        self._semtrk(semtrk)
        ds = semtrk.dsem
        ins = self.eng[q].dma_start(out=out, in_=in_)
        ds.dcount += 16
        ins.then_inc(ds.dsem, 16)
        ev = (ds.dsem, ds.dcount, ds, "dma")
        self._record(ev, reads, writes, part)
        return ev

    def allgather(self, src, dst, t_src, t_dst):
        self._deps("pool", [t_src], [t_dst], False)
        if self.cc is None:
            self.cc = Trk("ccsem")
            self.cc.dsem = self.nc.semaphore("ccsem").__enter__()
            self.dtrks.append(self.cc)
        ds = self.cc
        ins = self.nc.gpsimd.collective_compute("AllGather", ALU.bypass, replica_groups=[list(range(NCORES))],
                                                ins=[src.opt()], outs=[dst.opt()])
        ds.dcount += 1
        ins.then_inc(ds.dsem)
        ev = (ds.dsem, ds.dcount, ds, "dma")
        self._record(ev, [t_src], [t_dst], False)
        return ev

    def barrier(self):
        for e in self.ENG:
            for e2 in self.ENG:
                if e2 != e and self.ecount[e2] > 0:
                    self._wait(e, (self.esem[e2], self.ecount[e2], None, e2))
            for t in self.dtrks:
                if t.dcount > 0:
                    self._wait(e, (t.dsem, t.dcount, t, "dma"))


import os


class StopEmit(Exception):
    pass


def dbg_stop(tag):
    if os.environ.get("DBG_STOP") == tag:
        raise StopEmit(tag)


class W:
    pass


def setup_common(k, g):
    nc = k.nc
    g.pb = [k.ps("bank%d" % i, [128, 512], F32) for i in range(8)]
    g.tpb = [Trk("bank%d" % i) for i in range(8)]
    g.ident = k.sb("ident", [128, 128], F32)
    g.rotT = k.sb("rotT", [128, 128], F32)
    g.sel = k.sb("sel", [16, NEXP * 128], F32)
    g.ones_b = k.sb("ones_b", [128, 128], BF16)
    g.onesdiv_b = k.sb("onesdiv_b", [128, 128], BF16)
    g.t_const = Trk("const")
    k.dma("sp", g.ident[:, :], g.d_ident[:, :], [], [g.t_const], g.t_const)
    k.dma("sp", g.rotT[:, :], g.d_rotT[:, :], [], [g.t_const], g.t_const)
    k.dma("sp", g.sel[:, :], g.d_sel[:, :], [], [g.t_const], g.t_const)
    k.op("dve", [], [g.t_const], lambda: nc.vector.memset(g.ones_b[:, :], 1.0), part=True)
    k.op("dve", [], [g.t_const], lambda: nc.vector.memset(g.onesdiv_b[:, :], 1.0 / 128.0), part=True)


def gather_weight(k, g, name, shard, rows, cols):
    ws = k.dram("ws_" + name, [rows, cols], BF16)
    wf = k.dram("wf_" + name, [NCORES * rows, cols], BF16)
    t_ws, t_wf = Trk("ws_" + name), Trk("wf_" + name)
    k.dma("pool", ws[:, :], shard, [], [t_ws], t_ws, part=False)
    k.allgather(ws, wf, t_ws, t_wf)
    return wf, t_wf


def emit_mods(k, g):
    nc = k.nc
    with k.scope():
        cct = k.sb("cct", [64, 128], F32)
        cT = k.sb("cT", [128, 64], F32)
        bt = [k.sb("bt%d" % i, [2, 512], F32) for i in range(2)]
        res = [k.sb("res%d" % i, [2, 512], F32) for i in range(2)]
        wp = [k.sb("wp%d" % i, [128, NCH, 512], F32) for i in range(2)]
        t_cct, t_cT = Trk("cct"), Trk("cT")
        t_bt = [Trk("bt0"), Trk("bt1")]
        t_res = [Trk("res0"), Trk("res1")]
        t_wp = [Trk("wp0"), Trk("wp1")]
        pst, t_pst = g.pb[0], g.tpb[0]
        k.dma("sp", cct[:, :], g.d_cc.rearrange("v (ch p) -> (v ch) p", p=128), [], [t_cct], t_cct)
        k.op("pe", [t_cct, g.t_const], [t_pst], lambda: nc.tensor.transpose(pst[:, 0:64], cct[:, :], g.ident[0:64, 0:64]))
        k.op("act", [t_pst], [t_cT], lambda: nc.scalar.activation(out=cT[:, :], in_=pst[:, 0:64], func=AF.Silu))
        cTv = cT[:, :].rearrange("p (v ch) -> p ch v", v=2)
        i = 0
        for L in g.layers:
            for blk in range(MCOLS // 512):
                wb, tw = wp[i % 2], t_wp[i % 2]
                po, tpo = g.pb[1 + i % 2], g.tpb[1 + i % 2]
                btb, tbt, rsb, trs = bt[i % 2], t_bt[i % 2], res[i % 2], t_res[i % 2]
                i += 1
                cs = slice(blk * 512, (blk + 1) * 512)
                k.dma("sp", wb[:, :, :], g.d_adaw[g.li[L], :, cs].rearrange("(ch p) n -> p ch n", p=128), [], [tw], tw, part=False)
                k.dma("sp", btb[0:1, :], g.d_adab[g.li[L], 0:1, cs], [], [tbt], tbt)
                k.dma("sp", btb[1:2, :], g.d_adab[g.li[L], 0:1, cs], [], [tbt], tbt)
                for ch in range(NCH):
                    k.op("pe", [tw, t_cT], [tpo],
                         lambda ch=ch: nc.tensor.matmul(po[0:2, :], cTv[:, ch, :], wb[:, ch, :], start=(ch == 0), stop=(ch == NCH - 1)))
                k.op("dve", [tpo, tbt], [trs], lambda: nc.vector.tensor_tensor(out=rsb[:, :], in0=po[0:2, :], in1=btb[:, :], op=ALU.add))
                for v in range(2):
                    k.dma("sp", g.mods_part[v * 4 + L:v * 4 + L + 1, cs], rsb[v:v + 1, :], [trs], [g.t_mods_part], trs)
    k.allgather(g.mods_part, g.mods_all, g.t_mods_part, g.t_mods_all)


def mods_vec(g, v, L, kk):
    return g.mods_all.rearrange("(r v l) (k j) -> v l k r j", v=2, l=4, k=6)[v, L, kk]


def load_layer_mods(k, g, L, m):
    nc = k.nc
    m.modT = k.sb("modT", [128, 2, 6, 32], F32)
    m.t_modT = Trk("modT")
    with k.scope():
        rows = [k.sb("mrow%d" % i, [96, 128], F32) for i in range(4)]
        t_rows = [Trk("mrow%d" % i) for i in range(4)]
        for v in range(2):
            for half in range(2):
                rt, trt = rows[v * 2 + half], t_rows[v * 2 + half]
                for kl in range(3):
                    kk = half * 3 + kl
                    src = g.mods_all.rearrange("(r v l) (k c p) -> v l k r c p", v=2, l=4, k=6, c=4)[v, L, kk]
                    for r in range(8):
                        k.dma("sp", rt[kl * 32 + r * 4:kl * 32 + r * 4 + 4, :], src[r], [g.t_mods_all], [trt], trt)
                pb, tpb = g.pb[v * 2 + half], g.tpb[v * 2 + half]
                k.op("pe", [trt, g.t_const], [tpb], lambda pb=pb, rt=rt: nc.tensor.transpose(pb[:, 0:96], rt[:, :], g.ident[0:96, 0:96]))
                k.op("dve", [tpb], [m.t_modT],
                     lambda v=v, half=half, pb=pb: nc.vector.tensor_copy(out=m.modT[:, v, half * 3:half * 3 + 3, :],
                                                   in_=pb[:, 0:96].rearrange("p (a b) -> p a b", a=3)), part=True)
        for kk in (1, 4):
            k.op("dve", [m.t_modT], [m.t_modT],
                 lambda kk=kk: nc.vector.tensor_scalar_add(out=m.modT[:, :, kk, :], in0=m.modT[:, :, kk, :], scalar1=1.0))


def load_bcast(k, g, tile, trk, src_ap, rows=128):
    k.dma("sp", tile[0:rows, :].rearrange("p (r j) -> p r j", r=8), src_ap.partition_broadcast(rows), [g.t_mods_all], [trk], trk)


def emit_front(k, g, m, sub, src, t_src, hT, t_hT, ln=None, dst=None, t_dst=None, do_T=True, ntiles=9):
    nc = k.nc
    ish, isc = (0, 1) if sub == 0 else (3, 4)
    with k.scope():
        xt = [k.sb("xt%d" % i, [128, D], F32) for i in range(2)]
        t_xt = [Trk("xt0"), Trk("xt1")]
        if ln is not None:
            L, which = ln
            lng = k.sb("lng", [128, D], F32)
            lnb = k.sb("lnb", [128, D], F32)
            t_ln = Trk("lnp")
            k.dma("sp", lng[:, :], g.d_ln_g[L, which, :].partition_broadcast(128), [], [t_ln], t_ln)
            k.dma("sp", lnb[:, :], g.d_ln_b[L, which, :].partition_broadcast(128), [], [t_ln], t_ln)
            xn = [k.sb("xn%d" % i, [128, D], F32) for i in range(2)]
            t_xn = [Trk("xn0"), Trk("xn1")]
            st = k.sb("lnst", [128, 8, 6], F32)
            mv = k.sb("lnmv", [128, 2], F32)
            rstd = k.sb("lnrstd", [128, 1], F32)
            nb = k.sb("lnnb", [128, 1], F32)
            t_st = Trk("lnst")
        for ti, (t0, rows) in enumerate(TILES[:ntiles]):
            v = 0 if t0 < TX else 1
            xb, txb = xt[ti % 2], t_xt[ti % 2]
            k.dma("sp", xb[0:rows, :], src[t0:t0 + rows, :], [t_src], [txb], txb, part=False)
            cur, tcur = xb, txb
            if ln is not None:
                xo, txo = xn[ti % 2], t_xn[ti % 2]
                for c in range(8):
                    k.op("dve", [txb], [t_st], lambda c=c: nc.vector.bn_stats(out=st[0:rows, c, :], in_=xb[0:rows, c * 512:(c + 1) * 512]), part=(c > 0))
                k.op("dve", [t_st], [t_st], lambda: nc.vector.bn_aggr(out=mv[0:rows, :], in_=st[0:rows, :, :].rearrange("p a b -> p (a b)")), part=True)
                k.op("dve", [t_st], [t_st], lambda: nc.vector.tensor_scalar_add(out=rstd[0:rows, :], in0=mv[0:rows, 1:2], scalar1=LN_EPS), part=True)
                k.op("act", [t_st], [t_st], lambda: nc.scalar.activation(out=rstd[0:rows, :], in_=rstd[0:rows, :], func=AF.Sqrt), part=True)
                k.op("dve", [t_st], [t_st], lambda: nc.vector.reciprocal(out=rstd[0:rows, :], in_=rstd[0:rows, :]), part=True)
                k.op("dve", [t_st], [t_st], lambda: nc.vector.scalar_tensor_tensor(out=nb[0:rows, :], in0=mv[0:rows, 0:1], scalar=-1.0, in1=rstd[0:rows, :], op0=ALU.mult, op1=ALU.mult), part=True)
                k.op("act", [txb, t_st], [txo], lambda: nc.scalar.activation(out=xo[0:rows, :], in_=xb[0:rows, :], func=AF.Identity, scale=rstd[0:rows, 0:1], bias=nb[0:rows, 0:1]))
                k.op("dve", [txo, t_ln], [txo], lambda: nc.vector.tensor_tensor(out=xo[0:rows, :], in0=xo[0:rows, :], in1=lng[0:rows, :], op=ALU.mult))
                k.op("dve", [txo, t_ln], [txo], lambda: nc.vector.tensor_tensor(out=xo[0:rows, :], in0=xo[0:rows, :], in1=lnb[0:rows, :], op=ALU.add))
                k.dma("sp", dst[t0:t0 + rows, :], xo[0:rows, :], [txo], [t_dst], txo)
                cur, tcur = xo, txo
            if not do_T:
                continue
            for c4 in range(8):
                pb, tpb = g.pb[c4 % 2], g.tpb[c4 % 2]
                for j in range(4):
                    ch = c4 * 4 + j
                    k.op("pe", [tcur, g.t_const], [tpb],
                         lambda ch=ch, j=j: nc.tensor.transpose(pb[:, j * 128:j * 128 + rows], cur[0:rows, ch * 128:(ch + 1) * 128], g.ident[0:rows, 0:rows]), part=(j > 0))
                for j in range(4):
                    ch = c4 * 4 + j
                    k.op("act", [tpb, m.t_modT], [t_hT],
                         lambda ch=ch, j=j: nc.scalar.activation(out=hT[:, ch, t0:t0 + rows], in_=pb[:, j * 128:j * 128 + rows], func=AF.Identity,
                                                               scale=m.modT[:, v, isc, ch:ch + 1], bias=m.modT[:, v, ish, ch:ch + 1]), part=True)


def emit_post(k, g, m, L, actT, t_actT, wf, t_wf, kchunks_of_panel, wrow_of, gate_idx, xsrc, t_xsrc, bias_ap=None, scale_ap=None, wcol_of=None):
    nc = k.nc
    with k.scope():
        gp = [[k.sb("gp%d_%d" % (i, v), [128, 512], F32) for v in range(2)] for i in range(2)]
        t_gp = [Trk("gp0"), Trk("gp1")]
        if bias_ap is not None:
            bbp = [k.sb("pbias%d" % i, [128, 512], F32) for i in range(2)]
            scp = [k.sb("pscale%d" % i, [128, 512], F32) for i in range(2)]
        nk = len(kchunks_of_panel(0))
        wp = [k.sb("wpan%d" % i, [128, nk, 512], BF16) for i in range(2)]
        t_wp = [Trk("wpan0"), Trk("wpan1")]
        xb = [k.sb("xblk%d" % i, [128, 512], F32) for i in range(2)]
        t_xb = [Trk("xblk0"), Trk("xblk1")]
        tb = [k.sb("tblk%d" % i, [128, 512], F32) for i in range(2)]
        t_tb = [Trk("tblk0"), Trk("tblk1")]
        ub = [k.sb("ublk%d" % i, [128, 512], F32) for i in range(2)]
        t_ub = [Trk("ublk0"), Trk("ublk1")]
        it = 0
        for pn in range(8):
            w, tw = wp[pn % 2], t_wp[pn % 2]
            cs = slice(pn * 512, (pn + 1) * 512)
            chunks = kchunks_of_panel(pn)
            r0 = wrow_of(pn, 0)
            wc0 = pn * 512 if wcol_of is None else wcol_of(pn)
            gb, t_gb = gp[pn % 2], t_gp[pn % 2]
            for v in range(2):
                k.dma("sp", gb[v][:, :], mods_vec(g, v, L, gate_idx)[pn].partition_broadcast(128), [g.t_mods_all], [t_gb], t_gb, part=(v > 0))
            if bias_ap is not None:
                bb, sc = bbp[pn % 2], scp[pn % 2]
                k.dma("sp", bb[:, :], bias_ap[cs].partition_broadcast(128), [], [t_gb], t_gb)
                k.dma("sp", sc[:, :], scale_ap[cs].partition_broadcast(128), [], [t_gb], t_gb)
                for v in range(2):
                    k.op("dve", [t_gb], [t_gb], lambda v=v, gb=gb, sc=sc: nc.vector.tensor_tensor(out=gb[v][:, :], in0=gb[v][:, :], in1=sc[:, :], op=ALU.mult))
            k.dma("sp", w[:, :, :], wf[r0:r0 + nk * 128, wc0:wc0 + 512].rearrange("(ch p) n -> p ch n", p=128), [t_wf], [tw], tw, part=False)
            for (t0, rows) in TILES:
                v = 0 if t0 < TX else 1
                pb, tpb = g.pb[it % 4], g.tpb[it % 4]
                x_, tx_, t_, tt_, u_, tu_ = xb[it % 2], t_xb[it % 2], tb[it % 2], t_tb[it % 2], ub[it % 2], t_ub[it % 2]
                it += 1
                k.dma("sp", x_[0:rows, :], xsrc[t0:t0 + rows, cs], [t_xsrc], [tx_], tx_, part=False)
                for ci, ch in enumerate(chunks):
                    k.op("pe", [t_actT, tw], [tpb],
                         lambda ci=ci, ch=ch: nc.tensor.matmul(pb[0:rows, :], actT[:, ch, t0:t0 + rows], w[:, ci, :], start=(ci == 0), stop=(ci == nk - 1)))
                if bias_ap is not None:
                    k.op("dve", [tpb, t_gb], [tt_], lambda: nc.vector.tensor_tensor(out=t_[0:rows, :], in0=pb[0:rows, :], in1=bb[0:rows, :], op=ALU.add))
                    k.op("dve", [tt_, t_gb], [tt_], lambda: nc.vector.tensor_tensor(out=t_[0:rows, :], in0=t_[0:rows, :], in1=gb[v][0:rows, :], op=ALU.mult))
                else:
                    k.op("dve", [tpb, t_gb], [tt_], lambda: nc.vector.tensor_tensor(out=t_[0:rows, :], in0=pb[0:rows, :], in1=gb[v][0:rows, :], op=ALU.mult))
                k.op("dve", [tt_, tx_], [tu_], lambda: nc.vector.scalar_tensor_tensor(out=u_[0:rows, :], in0=x_[0:rows, :], scalar=DN_ALPHA, in1=t_[0:rows, :], op0=ALU.mult, op1=ALU.add))
                k.dma("sp", g.u[t0:t0 + rows, cs], u_[0:rows, :], [tu_], [g.t_u], tu_)


def emit_qk_proj(k, g, hT, t_hT, wf, t_wf, nheads, outT, t_outT, gain, t_gain, norm, wp, t_wp, scr):
    nc = k.nc
    for h4 in range(nheads // 4):
        w, tw = wp[h4 % 2], t_wp[h4 % 2]
        k.dma("sp", w[:, :, :], wf[:, h4 * 512:(h4 + 1) * 512].rearrange("(ch p) n -> p ch n", p=128), [t_wf], [tw], tw, part=False)
        for hh in range(4):
            h = h4 * 4 + hh
            for gi, (c0, n) in enumerate(TGROUPS):
                rope = c0 < TX
                it = scr.it
                scr.it += 1
                pq, tpq = g.pb[it % 2], g.tpb[it % 2]
                for ch in range(NCH):
                    k.op("pe", [t_hT, tw], [tpq],
                         lambda ch=ch: nc.tensor.matmul(pq[:, 0:n], w[:, ch, hh * 128:(hh + 1) * 128], hT[:, ch, c0:c0 + n], start=(ch == 0), stop=(ch == NCH - 1)))
                qn, tqn = scr.qn[it % 2], scr.t_qn[it % 2]
                if norm:
                    sq, tsq = scr.sq[it % 2], scr.t_sq[it % 2]
                    pss, tpss = g.pb[2 + it % 2], g.tpb[2 + it % 2]
                    rs, trs = scr.rs[it % 2], scr.t_rs[it % 2]
                    k.op("act", [tpq], [tsq], lambda: nc.scalar.activation(out=sq[:, 0:n], in_=pq[:, 0:n], func=AF.Square))
                    k.op("pe", [tsq, g.t_const], [tpss], lambda: nc.tensor.matmul(pss[:, 0:n], g.onesdiv_b[:, :], sq[:, 0:n], start=True, stop=True))
                    k.op("dve", [tpss], [trs], lambda: nc.vector.tensor_scalar_add(out=rs[:, 0:n], in0=pss[:, 0:n], scalar1=RMS_EPS))
                    k.op("act", [trs], [trs], lambda: nc.scalar.activation(out=rs[:, 0:n], in_=rs[:, 0:n], func=AF.Sqrt))
                    k.op("dve", [trs], [trs], lambda: nc.vector.reciprocal(out=rs[:, 0:n], in_=rs[:, 0:n]))
                    k.op("dve", [tpq, trs, t_gain], [tqn], lambda: nc.vector.scalar_tensor_tensor(out=qn[:, 0:n], in0=pq[:, 0:n], scalar=gain[:, 0:1], in1=rs[:, 0:n], op0=ALU.mult, op1=ALU.mult))
                else:
                    k.op("act", [tpq], [tqn], lambda: nc.scalar.copy(out=qn[:, 0:n], in_=pq[:, 0:n]))
                if rope:
                    pr, tpr = g.pb[4 + it % 2], g.tpb[4 + it % 2]
                    t1, tt1 = scr.t1[it % 2], scr.t_t1[it % 2]
                    k.op("pe", [tqn, g.t_const], [tpr], lambda: nc.tensor.matmul(pr[:, 0:n], g.rotT[:, :], qn[:, 0:n], start=True, stop=True))
                    k.op("dve", [tqn, g.t_rope], [tt1], lambda: nc.vector.tensor_tensor(out=t1[:, 0:n], in0=qn[:, 0:n], in1=g.cosT[:, c0:c0 + n], op=ALU.mult))
                    k.op("dve", [tpr, g.t_rope], [tqn], lambda: nc.vector.tensor_tensor(out=qn[:, 0:n], in0=pr[:, 0:n], in1=g.sinT[:, c0:c0 + n], op=ALU.mult))
                    k.op("dve", [tqn, tt1], [t_outT], lambda: nc.vector.tensor_tensor(out=outT[:, h, c0:c0 + n], in0=t1[:, 0:n], in1=qn[:, 0:n], op=ALU.add), part=True)
                else:
                    k.op("act", [tqn], [t_outT], lambda: nc.scalar.copy(out=outT[:, h, c0:c0 + n], in_=qn[:, 0:n]), part=True)


def alloc_qk_scratch(k):
    scr = W()
    scr.it = 0
    scr.qn = [k.sb("qn%d" % i, [128, 512], F32) for i in range(2)]
    scr.t_qn = [Trk("qn0"), Trk("qn1")]
    scr.sq = [k.sb("sq%d" % i, [128, 512], BF16) for i in range(2)]
    scr.t_sq = [Trk("sq0"), Trk("sq1")]
    scr.rs = [k.sb("rs%d" % i, [128, 512], F32) for i in range(2)]
    scr.t_rs = [Trk("rs0"), Trk("rs1")]
    scr.t1 = [k.sb("t1%d" % i, [128, 512], F32) for i in range(2)]
    scr.t_t1 = [Trk("t10"), Trk("t11")]
    return scr


def emit_v_proj(k, g, hT, t_hT, wf, t_wf, ncols, vpart, t_vpart, vw, wp, t_wp, vs, t_vs):
    nc = k.nc
    it = 0
    for pn in range(ncols // 512):
        w, tw = wp[pn % 2], t_wp[pn % 2]
        k.dma("sp", w[:, :, :], wf[:, pn * 512:(pn + 1) * 512].rearrange("(ch p) n -> p ch n", p=128), [t_wf], [tw], tw, part=False)
        nh = 512 // vw
        for j, (t0, rows) in enumerate(TILES):
            pb, tpb = g.pb[6 + it % 2], g.tpb[6 + it % 2]
            v_, tv_ = vs[it % 2], t_vs[it % 2]
            it += 1
            for ch in range(NCH):
                k.op("pe", [t_hT, tw], [tpb], lambda ch=ch: nc.tensor.matmul(pb[0:rows, :], hT[:, ch, t0:t0 + rows], w[:, ch, :], start=(ch == 0), stop=(ch == NCH - 1)))
            k.op("act", [tpb], [tv_], lambda: nc.scalar.copy(out=v_[0:rows, :], in_=pb[0:rows, :]))
            k.dma("sp", vpart[pn * nh:(pn + 1) * nh, 0:rows, j, :].rearrange("h p d -> p h d"), v_[0:rows, :].rearrange("p (h d) -> p h d", h=nh), [tv_], [t_vpart], tv_)


def attn_pass(k, g, kT, t_kT, vv, t_vv, nvs, q_rhs, t_q, ncols, chunks, scr, po_list, ps_sum, ctxview=None):
    nc = k.nc
    scale = 1.0 / math.sqrt(HD)
    n = len(chunks)

    def S(i):
        cid, rows = chunks[i]
        it = scr.sit + i
        pb, tpb = g.pb[it % 2], g.tpb[it % 2]
        out = pb[0:rows, 0:ncols]
        if ctxview is not None:
            out = out.rearrange("p (a b) -> p a b", a=ctxview)
        k.op("pe", [t_kT, t_q], [tpb], lambda: nc.tensor.matmul(out, kT(cid, rows), q_rhs, start=True, stop=True))

    S(0)
    for i in range(n):
        cid, rows = chunks[i]
        if i + 1 < n:
            S(i + 1)
        it = scr.sit + i
        pb, tpb = g.pb[it % 2], g.tpb[it % 2]
        pT, tpT = scr.pT[it % 3], scr.t_pT[it % 3]
        k.op("act", [tpb], [tpT], lambda: nc.scalar.activation(out=pT[0:rows, 0:ncols], in_=pb[0:rows, 0:ncols], func=AF.Exp, scale=scale))
        for vi in range(nvs):
            po, tpo = po_list[vi]
            k.op("pe", [tpT, t_vv], [tpo], lambda vi=vi, po=po: nc.tensor.matmul(po[:, 0:ncols], vv(cid, rows, vi), pT[0:rows, 0:ncols], start=(i == 0), stop=(i == n - 1)))
        pss, tpss = ps_sum
        k.op("pe", [tpT, g.t_const], [tpss], lambda: nc.tensor.matmul(pss[:, 0:ncols], g.ones_b[0:rows, :], pT[0:rows, 0:ncols], start=(i == 0), stop=(i == n - 1)))
    scr.sit += n


ALL_CHUNKS = [(r * 9 + j, 128 if j < 8 else 32) for r in range(8) for j in range(9)]
CTX_CHUNKS = [(r * 9 + 8, 32) for r in range(8)]


def load_kv_head(k, g, kall, t_kall, vall, t_vall, hk, hv, vw, kT_sb, t_kT, v_sb, t_v):
    nkh = g.cur_nkh
    nvh = g.cur_nvh
    if hk is not None:
        for r in range(8):
            row0 = r * nkh * 128 + hk * 128
            k.dma("sp", kT_sb[:, r, :], kall[row0:row0 + 128, :], [t_kall], [t_kT], t_kT, part=(r > 0))
    if hv is not None:
        for r in range(8):
            k.dma("sp", v_sb[:, r, :, :], vall[r * nvh + hv, :, :, :], [t_vall], [t_v], t_v, part=(r > 0))


def emit_gqa_layer(k, g, m, L, j, x_cur, t_x):
    nc = k.nc
    wts = g.wts[L]
    g.cur_nkh, g.cur_nvh = 8, 8
    kpart = k.dram("kpart%d" % L, [8 * 128, T], BF16)
    kall = k.dram("kall%d" % L, [8 * 8 * 128, T], BF16)
    vpart = k.dram("vpart%d" % L, [8, 128, 9, 128], BF16)
    vall = k.dram("vall%d" % L, [64, 128, 9, 128], BF16)
    qd = k.dram("qscr%d" % L, [32, 128, T], BF16)
    t_kpart, t_kall, t_vpart, t_vall, t_qd = Trk("kpart"), Trk("kall"), Trk("vpart"), Trk("vall"), Trk("qd")
    with k.scope():
        hT = k.sb("hT", [128, NCH, T], BF16)
        t_hT = Trk("hT")
        emit_front(k, g, m, 0, x_cur, t_x, hT, t_hT)
        with k.scope():
            wp = [k.sb("wq%d" % i, [128, NCH, 512], BF16) for i in range(2)]
            t_wp = [Trk("wq0"), Trk("wq1")]
            scr = alloc_qk_scratch(k)
            gq = k.sb("gq", [128, 2], F32)
            t_gq = Trk("gq")
            k.dma("sp", gq[:, 0:1], g.d_qn_g[j:j + 1, :].rearrange("a p -> p a"), [], [t_gq], t_gq)
            k.dma("sp", gq[:, 1:2], g.d_kn_g[j:j + 1, :].rearrange("a p -> p a"), [], [t_gq], t_gq)
            kT_loc = k.sb("kTloc", [128, 8, T], BF16)
            t_kTl = Trk("kTloc")
            vs = [k.sb("vs%d" % i, [128, 512], BF16) for i in range(2)]
            t_vs = [Trk("vs0"), Trk("vs1")]
            emit_qk_proj(k, g, hT, t_hT, wts.wk[0], wts.wk[1], 8, kT_loc, t_kTl, gq[:, 1:2], t_gq, True, wp, t_wp, scr)
            for h in range(8):
                k.dma("sp", kpart[h * 128:(h + 1) * 128, :], kT_loc[:, h, :], [t_kTl], [t_kpart], t_kTl)
            dbg_stop("k")
            k.allgather(kpart, kall, t_kpart, t_kall)
            dbg_stop("kg")
            emit_v_proj(k, g, hT, t_hT, wts.wv[0], wts.wv[1], 1024, vpart, t_vpart, 128, wp, t_wp, vs, t_vs)
            dbg_stop("v")
            k.allgather(vpart.rearrange("h p j d -> (h p) (j d)"), vall.rearrange("h p j d -> (h p) (j d)"), t_vpart, t_vall)
            qT = k.sb("qTst", [128, 4, T], BF16)
            t_qT = Trk("qTst")
            g.after_exchange(L)
            for h4 in range(8):
                wsub = wts.wq[0][:, h4 * 512:(h4 + 1) * 512]
                emit_qk_proj(k, g, hT, t_hT, wsub, wts.wq[1], 4, qT, t_qT, gq[:, 0:1], t_gq, True, wp, t_wp, scr)
                for hh in range(4):
                    k.dma("sp", qd[h4 * 4 + hh, :, :], qT[:, hh, :], [t_qT], [t_qd], t_qT)
                if os.environ.get("DBG_Q1"):
                    break
    dbg_stop("q")
    with k.scope():
        oT = k.sb("oT", [128, NCH, T], BF16)
        t_oT = Trk("oT")
        with k.scope():
            kT_sb = [k.sb("kTh%d" % i, [128, 8, T], BF16) for i in range(2)]
            v_sb = [k.sb("vh%d" % i, [128, 8, 9, 128], BF16) for i in range(2)]
            t_kT = [Trk("kTh0"), Trk("kTh1")]
            t_v = [Trk("vh0"), Trk("vh1")]
            q_sb = [k.sb("qh%d" % i, [128, 4, T], BF16) for i in range(2)]
            t_q = [Trk("qh0"), Trk("qh1")]
            scr = W()
            scr.sit = 0
            scr.pT = [k.sb("pT%d" % i, [128, 512], BF16) for i in range(3)]
            scr.t_pT = [Trk("pT%d" % i) for i in range(3)]
            rsb = [k.sb("rsum%d" % i, [128, 512], F32) for i in range(2)]
            t_rsb = [Trk("rsum0"), Trk("rsum1")]
            grp = 0
            for kv in range(8):
                b = kv % 2
                load_kv_head(k, g, kall, t_kall, vall, t_vall, kv, kv, 128, kT_sb[b], t_kT[b], v_sb[b], t_v[b])
                for hh in range(4):
                    k.dma("sp", q_sb[b][:, hh, :], qd[kv * 4 + hh, :, :], [t_qd], [t_q[b]], t_q[b], part=(hh > 0))
                kTf = lambda cid, rows, b=b: kT_sb[b][:, cid // 9, (cid % 9) * 128:(cid % 9) * 128 + rows]
                vf = lambda cid, rows, vi, b=b: v_sb[b][0:rows, cid // 9, cid % 9, :]
                for hh in range(4):
                    h = kv * 4 + hh
                    for half in range(2):
                        c0 = half * 512
                        po = (g.pb[2 + grp % 2], g.tpb[2 + grp % 2])
                        pss = (g.pb[4 + grp % 2], g.tpb[4 + grp % 2])
                        r_, tr_ = rsb[grp % 2], t_rsb[grp % 2]
                        grp += 1
                        attn_pass(k, g, kTf, t_kT[b], vf, t_v[b], 1, q_sb[b][:, hh, c0:c0 + 512], t_q[b], 512, ALL_CHUNKS, scr, [po], pss)
                        k.op("dve", [pss[1]], [tr_], lambda: nc.vector.reciprocal(out=r_[:, :], in_=pss[0][:, :]))
                        k.op("dve", [po[1], tr_], [t_oT], lambda: nc.vector.tensor_tensor(out=oT[:, h, c0:c0 + 512], in0=po[0][:, :], in1=r_[:, :], op=ALU.mult), part=True)
                po = (g.pb[2 + grp % 2], g.tpb[2 + grp % 2])
                pss = (g.pb[4 + grp % 2], g.tpb[4 + grp % 2])
                r_, tr_ = rsb[grp % 2], t_rsb[grp % 2]
                grp += 1
                attn_pass(k, g, kTf, t_kT[b], vf, t_v[b], 1, q_sb[b][:, :, TX:T], t_q[b], 128, CTX_CHUNKS, scr, [po], pss, ctxview=4)
                k.op("dve", [pss[1]], [tr_], lambda: nc.vector.reciprocal(out=r_[:, 0:128], in_=pss[0][:, 0:128]))
                k.op("dve", [po[1], tr_], [t_oT],
                     lambda: nc.vector.tensor_tensor(out=oT[:, kv * 4:(kv + 1) * 4, TX:T], in0=po[0][:, 0:128].rearrange("p (a b) -> p a b", a=4),
                                                     in1=r_[:, 0:128].rearrange("p (a b) -> p a b", a=4), op=ALU.mult), part=True)
        dbg_stop("attn")
        emit_post(k, g, m, L, oT, t_oT, wts.wo[0], wts.wo[1], lambda pn: list(range(NCH)), lambda pn, ci: ci * 128, 2, x_cur, t_x)


def emit_router(k, g, L, hT, t_hT, gT, t_gT):
    nc = k.nc
    with k.scope():
        wr = k.sb("wr", [128, NCH, 20], BF16)
        t_wr = Trk("wr")
        k.dma("pool", wr[:, :, :], g.d_wr[L].rearrange("(ch p) n -> p ch n", p=128), [], [t_wr], t_wr, part=False)
        rb = k.sb("rb", [128, 20], F32)
        k.dma("sp", rb[:, :], g.d_rb[L, 0, :].partition_broadcast(128), [], [t_wr], t_wr)
        S = {}
        for nm, w_ in (("lg", 20), ("gmax", 1), ("ngmax", 1), ("ohg", 4), ("ex", 4), ("se", 1), ("pg", 1), ("esel", 4), ("m1", 1), ("mask1", 4),
                       ("e2", 4), ("m2", 1), ("mask2", 4), ("dm", 1), ("ed", 1), ("den", 1), ("w1", 1), ("w1p", 1), ("w2p", 1), ("gsel", 4), ("gate", 16)):
            S[nm] = k.sb("r_" + nm, [128, w_], F32)
        tS = Trk("rscr")
        for ti, (t0, rows) in enumerate(TILES):
            pb, tpb = g.pb[ti % 2], g.tpb[ti % 2]
            pt, tpt = g.pb[2 + ti % 2], g.tpb[2 + ti % 2]
            R = slice(0, rows)
            for ch in range(NCH):
                k.op("pe", [t_hT, t_wr], [tpb], lambda ch=ch: nc.tensor.matmul(pb[R, 0:20], hT[:, ch, t0:t0 + rows], wr[:, ch, :], start=(ch == 0), stop=(ch == NCH - 1)))
            V = nc.vector
            ops = [
                ("dve", lambda: V.tensor_tensor(out=S["lg"][R, :], in0=pb[R, 0:20], in1=rb[R, :], op=ALU.add)),
                ("dve", lambda: V.reduce_max(out=S["gmax"][R, :], in_=S["lg"][R, 0:4], axis=AX.X)),
                ("dve", lambda: V.tensor_scalar(out=S["ohg"][R, :], in0=S["lg"][R, 0:4], scalar1=S["gmax"][R, 0:1], scalar2=None, op0=ALU.is_equal)),
                ("dve", lambda: V.tensor_scalar(out=S["ngmax"][R, :], in0=S["gmax"][R, :], scalar1=-1.0, scalar2=None, op0=ALU.mult)),
                ("act", lambda: nc.scalar.activation(out=S["ex"][R, :], in_=S["lg"][R, 0:4], func=AF.Exp, bias=S["ngmax"][R, 0:1], scale=1.0, accum_out=S["se"][R, 0:1])),
                ("dve", lambda: V.reciprocal(out=S["pg"][R, :], in_=S["se"][R, :])),
                ("dve", lambda: V.tensor_scalar(out=S["esel"][R, :], in0=S["lg"][R, 4:8], scalar1=S["ohg"][R, 0:1], scalar2=None, op0=ALU.mult)),
            ]
            for gi in range(1, 4):
                ops.append(("dve", lambda gi=gi: V.scalar_tensor_tensor(out=S["esel"][R, :], in0=S["lg"][R, 4 + 4 * gi:8 + 4 * gi], scalar=S["ohg"][R, gi:gi + 1],
                                                                        in1=S["esel"][R, :], op0=ALU.mult, op1=ALU.add)))
            ops += [
                ("dve", lambda: V.reduce_max(out=S["m1"][R, :], in_=S["esel"][R, :], axis=AX.X)),
                ("dve", lambda: V.tensor_scalar(out=S["mask1"][R, :], in0=S["esel"][R, :], scalar1=S["m1"][R, 0:1], scalar2=None, op0=ALU.is_equal)),
                ("dve", lambda: V.scalar_tensor_tensor(out=S["e2"][R, :], in0=S["mask1"][R, :], scalar=-1e30, in1=S["esel"][R, :], op0=ALU.mult, op1=ALU.add)),
                ("dve", lambda: V.reduce_max(out=S["m2"][R, :], in_=S["e2"][R, :], axis=AX.X)),
                ("dve", lambda: V.tensor_scalar(out=S["mask2"][R, :], in0=S["e2"][R, :], scalar1=S["m2"][R, 0:1], scalar2=None, op0=ALU.is_equal)),
                ("dve", lambda: V.tensor_tensor(out=S["dm"][R, :], in0=S["m2"][R, :], in1=S["m1"][R, :], op=ALU.subtract)),
                ("act", lambda: nc.scalar.activation(out=S["ed"][R, :], in_=S["dm"][R, :], func=AF.Exp)),
                ("dve", lambda: V.tensor_scalar_add(out=S["den"][R, :], in0=S["ed"][R, :], scalar1=1.0)),
                ("dve", lambda: V.reciprocal(out=S["w1"][R, :], in_=S["den"][R, :])),
                ("dve", lambda: V.tensor_tensor(out=S["w1p"][R, :], in0=S["w1"][R, :], in1=S["pg"][R, :], op=ALU.mult)),
                ("dve", lambda: V.tensor_tensor(out=S["w2p"][R, :], in0=S["ed"][R, :], in1=S["w1p"][R, :], op=ALU.mult)),
                ("dve", lambda: V.tensor_scalar(out=S["gsel"][R, :], in0=S["mask1"][R, :], scalar1=S["w1p"][R, 0:1], scalar2=None, op0=ALU.mult)),
                ("dve", lambda: V.scalar_tensor_tensor(out=S["gsel"][R, :], in0=S["mask2"][R, :], scalar=S["w2p"][R, 0:1], in1=S["gsel"][R, :], op0=ALU.mult, op1=ALU.add)),
            ]
            for gi in range(4):
                ops.append(("dve", lambda gi=gi: V.tensor_scalar(out=S["gate"][R, 4 * gi:4 * gi + 4], in0=S["gsel"][R, :], scalar1=S["ohg"][R, gi:gi + 1], scalar2=None, op0=ALU.mult)))
            first = True
            for e, fn in ops:
                k.op(e, [tS, tpb, t_wr] if first else [tS], [tS], fn)
                first = False
            k.op("pe", [tS, g.t_const], [tpt], lambda: nc.tensor.transpose(pt[0:16, 0:rows], S["gate"][R, :], g.ident[R, R]))
            k.op("act", [tpt], [t_gT], lambda: nc.scalar.copy(out=gT[:, t0:t0 + rows], in_=pt[0:16, 0:rows]), part=True)


def emit_moe_up(k, g, m, L, hT, t_hT):
    nc = k.nc
    wts = g.wts[L]
    hid_d = g.hid_d
    with k.scope():
        gT = k.sb("gT", [16, T], F32)
        t_gT = Trk("gT")
        emit_router(k, g, L, hT, t_hT, gT, t_gT)
        w1 = [k.sb("w1_%d" % i, [128, NCH, DEXP], BF16) for i in range(2)]
        w3 = [k.sb("w3_%d" % i, [128, NCH, DEXP], BF16) for i in range(2)]
        t_w1 = [Trk("w1_0"), Trk("w1_1")]
        t_w3 = [Trk("w3_0"), Trk("w3_1")]
        sa = [k.sb("sa%d" % i, [128, 512], F32) for i in range(2)]
        t_sa = [Trk("sa0"), Trk("sa1")]
        hs = [k.sb("hs%d" % i, [128, 512], BF16) for i in range(3)]
        t_hs = [Trk("hs%d" % i) for i in range(3)]
        it = 0
        for e in range(NEXP):
            a_, ta_, b_, tb_ = w1[e % 2], t_w1[e % 2], w3[e % 2], t_w3[e % 2]
            k.dma("sp", a_[:, :, :], wts.w1[0][e * D:(e + 1) * D, :].rearrange("(ch p) f -> p ch f", p=128), [wts.w1[1]], [ta_], ta_, part=False)
            k.dma("sp", b_[:, :, :], wts.w3[0][e * D:(e + 1) * D, :].rearrange("(ch p) f -> p ch f", p=128), [wts.w3[1]], [tb_], tb_, part=False)
            for (c0, n) in TGROUPS:
                for fc in range(3):
                    pa, tpa = g.pb[it % 2], g.tpb[it % 2]
                    pu, tpu = g.pb[2 + it % 2], g.tpb[2 + it % 2]
                    pg, tpg = g.pb[4 + it % 2], g.tpb[4 + it % 2]
                    s_, ts_ = sa[it % 2], t_sa[it % 2]
                    h_, th_ = hs[it % 3], t_hs[it % 3]
                    it += 1
                    fs = slice(fc * 128, (fc + 1) * 128)
                    for ch in range(NCH):
                        k.op("pe", [t_hT, ta_], [tpa], lambda ch=ch: nc.tensor.matmul(pa[:, 0:n], a_[:, ch, fs], hT[:, ch, c0:c0 + n], start=(ch == 0), stop=(ch == NCH - 1)))
                    for ch in range(NCH):
                        k.op("pe", [t_hT, tb_], [tpu], lambda ch=ch: nc.tensor.matmul(pu[:, 0:n], b_[:, ch, fs], hT[:, ch, c0:c0 + n], start=(ch == 0), stop=(ch == NCH - 1)))
                    k.op("pe", [t_gT, g.t_const], [tpg], lambda: nc.tensor.matmul(pg[:, 0:n], g.sel[:, e * 128:(e + 1) * 128], gT[:, c0:c0 + n], start=True, stop=True))
                    k.op("act", [tpa], [ts_], lambda: nc.scalar.activation(out=s_[:, 0:n], in_=pa[:, 0:n], func=AF.Silu))
                    k.op("dve", [ts_, tpu], [ts_], lambda: nc.vector.tensor_tensor(out=s_[:, 0:n], in0=s_[:, 0:n], in1=pu[:, 0:n], op=ALU.mult))
                    k.op("dve", [ts_, tpg], [th_], lambda: nc.vector.tensor_tensor(out=h_[:, 0:n], in0=s_[:, 0:n], in1=pg[:, 0:n], op=ALU.mult))
                    k.dma("sp", hid_d[e * 3 + fc, :, c0:c0 + n], h_[:, 0:n], [th_], [g.t_hid], th_)


def emit_moe_down(k, g, m, L, xsrc, t_xsrc):
    nc = k.nc
    wts = g.wts[L]
    hid_d = g.hid_d
    with k.scope():
        gp = [[k.sb("gp%d_%d" % (i, v), [128, 512], F32) for v in range(2)] for i in range(2)]
        t_gp = [Trk("gp0"), Trk("gp1")]
        hh = k.sb("hidh", [128, 48, 544], BF16)
        t_hh = Trk("hidh")
        wp = [k.sb("w2p%d" % i, [128, 48, 512], BF16) for i in range(2)]
        t_wp = [Trk("w2p0"), Trk("w2p1")]
        xb = [k.sb("xblk%d" % i, [128, 512], F32) for i in range(2)]
        t_xb = [Trk("xblk0"), Trk("xblk1")]
        tb = [k.sb("tblk%d" % i, [128, 512], F32) for i in range(2)]
        t_tb = [Trk("tblk0"), Trk("tblk1")]
        ub = [k.sb("ublk%d" % i, [128, 512], F32) for i in range(2)]
        t_ub = [Trk("ublk0"), Trk("ublk1")]
        it = 0
        ip = 0
        for half in range(2):
            hc0, hn = (0, 512) if half == 0 else (512, 544)
            k.dma("sp", hh[:, :, 0:hn], hid_d[:, :, hc0:hc0 + hn].rearrange("c p t -> p c t"), [g.t_hid], [t_hh], t_hh, part=False)
            tiles = TILES[0:4] if half == 0 else TILES[4:9]
            for pn in range(8):
                w, tw = wp[ip % 2], t_wp[ip % 2]
                gb, t_gb = gp[ip % 2], t_gp[ip % 2]
                ip += 1
                cs = slice(pn * 512, (pn + 1) * 512)
                for v in range(2):
                    k.dma("sp", gb[v][:, :], mods_vec(g, v, L, 5)[pn].partition_broadcast(128), [g.t_mods_all], [t_gb], t_gb, part=(v > 0))
                k.dma("sp", w[:, :, :], wts.w2[0][:, cs].rearrange("(c p) n -> p c n", p=128), [wts.w2[1]], [tw], tw, part=False)
                for (t0, rows) in tiles:
                    v = 0 if t0 < TX else 1
                    pb, tpb = g.pb[it % 4], g.tpb[it % 4]
                    x_, tx_, t_, tt_, u_, tu_ = xb[it % 2], t_xb[it % 2], tb[it % 2], t_tb[it % 2], ub[it % 2], t_ub[it % 2]
                    it += 1
                    k.dma("sp", x_[0:rows, :], xsrc[t0:t0 + rows, cs], [t_xsrc], [tx_], tx_, part=False)
                    for c in range(48):
                        k.op("pe", [t_hh, tw], [tpb], lambda c=c: nc.tensor.matmul(pb[0:rows, :], hh[:, c, t0 - hc0:t0 - hc0 + rows], w[:, c, :], start=(c == 0), stop=(c == 47)))
                    k.op("dve", [tpb, t_gb], [tt_], lambda: nc.vector.tensor_tensor(out=t_[0:rows, :], in0=pb[0:rows, :], in1=gb[v][0:rows, :], op=ALU.mult))
                    k.op("dve", [tt_, tx_], [tu_], lambda: nc.vector.scalar_tensor_tensor(out=u_[0:rows, :], in0=x_[0:rows, :], scalar=DN_ALPHA, in1=t_[0:rows, :], op0=ALU.mult, op1=ALU.add))
                    k.dma("sp", g.u[t0:t0 + rows, cs], u_[0:rows, :], [tu_], [g.t_u], tu_)


EXT = 1088


def emit_pool_layer(k, g, m, L, x_cur, t_x):
    nc = k.nc
    wts = g.wts[L]
    bpart = k.dram("bpart", [32, D], F32)
    ball = k.dram("ball", [256, D], F32)
    halo = k.dram("halo", [32, D], F32)
    t_bpart, t_ball, t_halo = Trk("bpart"), Trk("ball"), Trk("halo")
    for i, r0 in enumerate((0, TX - 8, TX, T - 8)):
        k.dma("sp", bpart[i * 8:(i + 1) * 8, :], x_cur[r0:r0 + 8, :], [t_x], [t_bpart], t_bpart)
    k.allgather(bpart, ball, t_bpart, t_ball)
    g.after_exchange(L)
    with k.scope():
        ball_sb = k.sb("ball_sb", [128, 2, D], F32)
        hsel = k.sb("hsel", [128, 2, 32], F32)
        halo_sb = k.sb("halo_sb", [32, D], F32)
        t_b, t_h = Trk("ball_sb"), Trk("halo_sb")
        k.dma("sp", ball_sb[:, :, :], ball.rearrange("(c p) n -> p c n", p=128), [t_ball], [t_b], t_b)
        k.dma("sp", hsel[:, :, :], g.d_hsel.rearrange("(c p) n -> p c n", p=128), [], [t_b], t_b)
        for pn in range(8):
            pb, tpb = g.pb[pn % 2], g.tpb[pn % 2]
            for c in range(2):
                k.op("pe", [t_b], [tpb], lambda c=c: nc.tensor.matmul(pb[0:32, :], hsel[:, c, :], ball_sb[:, c, pn * 512:(pn + 1) * 512], start=(c == 0), stop=(c == 1)))
            k.op("act", [tpb], [t_h], lambda: nc.scalar.copy(out=halo_sb[:, pn * 512:(pn + 1) * 512], in_=pb[0:32, :]), part=True)
        k.dma("sp", halo[:, :], halo_sb[:, :], [t_h], [t_halo], t_h)
    with k.scope():
        yT = k.sb("yT", [128, NCH, T], BF16)
        t_yT = Trk("yT")
        with k.scope():
            hTe = k.sb("hTe", [128, 8, EXT], F32)
            t_hTe = Trk("hTe")
            xt = [k.sb("xt%d" % i, [128, D], F32) for i in range(2)]
            t_xt = [Trk("xt0"), Trk("xt1")]
            inv = k.sb("pinv", [128, 4 * T], F32)
            hmask = k.sb("hmask", [128, 32], F32)
            t_tab = Trk("ptab")
            k.dma("sp", inv[:, :], g.d_pinv[0, :].partition_broadcast(128), [], [t_tab], t_tab)
            k.dma("sp", hmask[:, :], g.d_hmask[0, :].partition_broadcast(128), [], [t_tab], t_tab)
            s2 = k.sb("s2", [128, 1040], F32)
            s4 = k.sb("s4", [128, 1040], F32)
            s8 = k.sb("s8", [128, 1040], F32)
            s16 = k.sb("s16", [128, 1040], F32)
            t_s = Trk("pools")
            tiles = [(DRAMX, t0, rows, [(0, rows, 8 + t0 if t0 < TX else 1048, 0 if t0 < TX else 1)]) for (t0, rows) in TILES]
            tiles.append((DRAMH, 0, 32, [(0, 8, 0, 0), (8, 8, 1032, 0), (16, 8, 1040, 1), (24, 8, 1080, 1)]))
            it = 0
            for half in range(4):
                for ti, (srcid, t0, rows, places) in enumerate(tiles):
                    xb, txb = xt[it % 2], t_xt[it % 2]
                    it += 1
                    if srcid == DRAMX:
                        k.dma("sp", xb[0:rows, :], x_cur[t0:t0 + rows, :], [t_x], [txb], txb, part=False)
                    else:
                        k.dma("sp", xb[0:rows, :], halo[0:rows, :], [t_halo], [txb], txb, part=False)
                    for c4 in range(2):
                        pb, tpb = g.pb[c4 % 2], g.tpb[c4 % 2]
                        for j in range(4):
                            ch = half * 8 + c4 * 4 + j
                            k.op("pe", [txb, g.t_const], [tpb],
                                 lambda ch=ch, j=j: nc.tensor.transpose(pb[:, j * 128:j * 128 + rows], xb[0:rows, ch * 128:(ch + 1) * 128], g.ident[0:rows, 0:rows]), part=(j > 0))
                        for j in range(4):
                            ch = half * 8 + c4 * 4 + j
                            cl = c4 * 4 + j
                            for (sr, n, dc, v) in places:
                                k.op("act", [tpb, m.t_modT], [t_hTe],
                                     lambda ch=ch, j=j, cl=cl, sr=sr, n=n, dc=dc, v=v: nc.scalar.activation(out=hTe[:, cl, dc:dc + n], in_=pb[:, j * 128 + sr:j * 128 + sr + n], func=AF.Identity,
                                                                                     scale=m.modT[:, v, 1, ch:ch + 1], bias=m.modT[:, v, 0, ch:ch + 1]), part=True)
                for cl in range(8):
                    for (dc, mc) in ((0, 0), (1032, 8), (1040, 16), (1080, 24)):
                        k.op("dve", [t_hTe, t_tab], [t_hTe], lambda cl=cl, dc=dc, mc=mc: nc.vector.tensor_tensor(out=hTe[:, cl, dc:dc + 8], in0=hTe[:, cl, dc:dc + 8], in1=hmask[:, mc:mc + 8], op=ALU.mult), part=True)
                for cl in range(8):
                    ch = half * 8 + cl
                    gi = ch // 8
                    for (e0, n, tok0) in ((0, 1040, 0), (1040, 48, TX)):
                        E = lambda a, b, cl=cl, e0=e0: hTe[:, cl, e0 + a:e0 + b]
                        V = nc.vector
                        k.op("dve", [t_hTe], [t_s], lambda: V.tensor_tensor(out=s2[:, 1:n], in0=E(0, n - 1), in1=E(1, n), op=ALU.add))
                        win = s2
                        if gi >= 1:
                            k.op("dve", [t_s], [t_s], lambda: V.tensor_tensor(out=s4[:, 2:n - 1], in0=s2[:, 1:n - 2], in1=s2[:, 3:n], op=ALU.add))
                            win = s4
                        if gi >= 2:
                            k.op("dve", [t_s], [t_s], lambda: V.tensor_tensor(out=s8[:, 4:n - 3], in0=s4[:, 2:n - 5], in1=s4[:, 6:n - 1], op=ALU.add))
                            win = s8
                        if gi >= 3:
                            k.op("dve", [t_s], [t_s], lambda: V.tensor_tensor(out=s16[:, 8:n - 7], in0=s8[:, 4:n - 11], in1=s8[:, 12:n - 3], op=ALU.add))
                            win = s16
                        nt = n - 16
                        k.op("dve", [t_s, t_tab], [t_s], lambda win=win: V.tensor_tensor(out=win[:, 8:8 + nt], in0=win[:, 8:8 + nt], in1=inv[:, gi * T + tok0:gi * T + tok0 + nt], op=ALU.mult))
                        k.op("dve", [t_s, t_hTe], [t_yT], lambda win=win: V.tensor_tensor(out=yT[:, ch, tok0:tok0 + nt], in0=win[:, 8:8 + nt], in1=E(8, 8 + nt), op=ALU.subtract), part=True)
        emit_post(k, g, m, L, yT, t_yT, wts.pw[0], wts.pw[1], lambda pn: list(range((pn // 2) * 8, (pn // 2) * 8 + 8)), lambda pn, ci: (pn // 2) * 1024 + ci * 128, 2, x_cur, t_x,
                  bias_ap=g.d_pool_b[0, :], scale_ap=g.d_pool_scale[0, :], wcol_of=lambda pn: (pn % 2) * 512)


DRAMX, DRAMH = 0, 1


def emit_diff_layer(k, g, m, L, x_cur, t_x):
    nc = k.nc
    V = nc.vector
    lam_init = 0.8 - 0.6 * math.exp(-0.3 * L)
    wts = g.wts[L]
    g.cur_nkh, g.cur_nvh = 32, 16
    kpart = k.dram("kpart%d" % L, [32 * 128, T], BF16)
    kall = k.dram("kall%d" % L, [8 * 32 * 128, T], BF16)
    vpart = k.dram("vpart%d" % L, [16, 128, 9, 256], BF16)
    vall = k.dram("vall%d" % L, [128, 128, 9, 256], BF16)
    qd = k.dram("qscr%d" % L, [32, 128, T], BF16)
    t_kpart, t_kall, t_vpart, t_vall, t_qd = Trk("kpart"), Trk("kall"), Trk("vpart"), Trk("vall"), Trk("qd")
    with k.scope():
        hT = k.sb("hT", [128, NCH, T], BF16)
        t_hT = Trk("hT")
        emit_front(k, g, m, 0, x_cur, t_x, hT, t_hT)
        with k.scope():
            wp = [k.sb("wq%d" % i, [128, NCH, 512], BF16) for i in range(2)]
            t_wp = [Trk("wq0"), Trk("wq1")]
            scr = alloc_qk_scratch(k)
            stg = k.sb("qkst", [128, 4, T], BF16)
            t_stg = Trk("qkst")
            vs = [k.sb("vs%d" % i, [128, 512], BF16) for i in range(2)]
            t_vs = [Trk("vs0"), Trk("vs1")]
            for h4 in range(8):
                emit_qk_proj(k, g, hT, t_hT, wts.dk[0][:, h4 * 512:(h4 + 1) * 512], wts.dk[1], 4, stg, t_stg, None, None, False, wp, t_wp, scr)
                for hh in range(4):
                    k.dma("sp", kpart[(h4 * 4 + hh) * 128:(h4 * 4 + hh + 1) * 128, :], stg[:, hh, :], [t_stg], [t_kpart], t_stg)
            k.allgather(kpart, kall, t_kpart, t_kall)
            emit_v_proj(k, g, hT, t_hT, wts.dv[0], wts.dv[1], D, vpart, t_vpart, 256, wp, t_wp, vs, t_vs)
            k.allgather(vpart.rearrange("h p j d -> (h p) (j d)"), vall.rearrange("h p j d -> (h p) (j d)"), t_vpart, t_vall)
            g.after_exchange(L)
            for h4 in range(8):
                emit_qk_proj(k, g, hT, t_hT, wts.dq[0][:, h4 * 512:(h4 + 1) * 512], wts.dq[1], 4, stg, t_stg, None, None, False, wp, t_wp, scr)
                for hh in range(4):
                    k.dma("sp", qd[h4 * 4 + hh, :, :], stg[:, hh, :], [t_stg], [t_qd], t_stg)
    with k.scope():
        oT = k.sb("oT", [128, NCH, T], BF16)
        t_oT = Trk("oT")
        with k.scope():
            lamt = k.sb("lamt", [128, 512], F32)
            lp = k.sb("lamp", [128, 256], F32)
            ls = k.sb("lams", [128, 4], F32)
            gsc = k.sb("gsc", [128, 2], F32)
            t_lam = Trk("lam")
            k.dma("sp", lamt[:, :], g.d_dlam.rearrange("a b -> (a b)").partition_broadcast(128), [], [t_lam], t_lam)
            for s_ in range(2):
                k.dma("sp", gsc[:, s_:s_ + 1], g.d_subln[0:1, s_ * 128:(s_ + 1) * 128].rearrange("a p -> p a"), [], [t_lam], t_lam)
            for i in range(2):
                k.op("dve", [t_lam], [t_lam], lambda i=i: V.tensor_tensor(out=lp[:, i * 128:(i + 1) * 128], in0=lamt[:, i * 256:i * 256 + 128], in1=lamt[:, i * 256 + 128:i * 256 + 256], op=ALU.mult))
                k.op("dve", [t_lam], [t_lam], lambda i=i: V.reduce_sum(out=ls[:, i:i + 1], in_=lp[:, i * 128:(i + 1) * 128], axis=AX.X))
            k.op("act", [t_lam], [t_lam], lambda: nc.scalar.activation(out=ls[:, 0:2], in_=ls[:, 0:2], func=AF.Exp))
            k.op("dve", [t_lam], [t_lam], lambda: V.tensor_tensor(out=ls[:, 2:3], in0=ls[:, 0:1], in1=ls[:, 1:2], op=ALU.subtract))
            k.op("dve", [t_lam], [t_lam], lambda: V.tensor_scalar_add(out=ls[:, 3:4], in0=ls[:, 2:3], scalar1=lam_init))
            k.op("dve", [t_lam], [t_lam], lambda: V.tensor_scalar(out=gsc[:, :], in0=gsc[:, :], scalar1=1.0 - lam_init, scalar2=None, op0=ALU.mult))
            lam = ls[:, 3:4]
            kT_sb = [k.sb("kTh%d" % i, [128, 8, T], BF16) for i in range(2)]
            t_kT = [Trk("kTh0"), Trk("kTh1")]
            v_sb = k.sb("vh", [128, 8, 9, 256], BF16)
            t_v = Trk("vh")
            q_sb = [k.sb("qh%d" % i, [128, 2, T], BF16) for i in range(2)]
            t_q = [Trk("qh0"), Trk("qh1")]
            scr = W()
            scr.sit = 0
            scr.pT = [k.sb("pT%d" % i, [128, 512], BF16) for i in range(3)]
            scr.t_pT = [Trk("pT%d" % i) for i in range(3)]
            r1 = k.sb("r1", [128, 512], F32)
            r2 = k.sb("r2", [128, 512], F32)
            A = [k.sb("A%d" % i, [128, 512], F32) for i in range(2)]
            B = k.sb("Bt", [128, 512], F32)
            sq = [k.sb("dsq%d" % i, [128, 512], BF16) for i in range(2)]
            rr = k.sb("rr", [128, 512], F32)
            t_c = Trk("dcomb")
            vf = lambda cid, rows, vi: v_sb[0:rows, cid // 9, cid % 9, vi * 128:(vi + 1) * 128]
            for h in range(16):
                b = h % 2
                load_kv_head(k, g, kall, t_kall, vall, t_vall, None, h, 256, None, None, v_sb, t_v)
                for wh in range(2):
                    k.dma("sp", q_sb[b][:, wh, :], qd[2 * h + wh, :, :], [t_qd], [t_q[b]], t_q[b], part=(wh > 0))
                for wh in range(2):
                    load_kv_head(k, g, kall, t_kall, vall, t_vall, 2 * h + wh, None, 256, kT_sb[wh], t_kT[wh], None, None)
                for (c0, n, chunks) in ((0, 512, ALL_CHUNKS), (512, 512, ALL_CHUNKS), (TX, 32, CTX_CHUNKS)):
                    for wh in range(2):
                        kTf = lambda cid, rows, wh=wh: kT_sb[wh][:, cid // 9, (cid % 9) * 128:(cid % 9) * 128 + rows]
                        po = [(g.pb[2 + 2 * wh], g.tpb[2 + 2 * wh]), (g.pb[3 + 2 * wh], g.tpb[3 + 2 * wh])]
                        pss = (g.pb[6 + wh], g.tpb[6 + wh])
                        attn_pass(k, g, kTf, t_kT[wh], vf, t_v, 2, q_sb[b][:, wh, c0:c0 + n], t_q[b], n, chunks, scr, po, pss)
                    N = slice(0, n)
                    k.op("dve", [g.tpb[6]], [t_c], lambda: V.reciprocal(out=r1[:, N], in_=g.pb[6][:, N]))
                    k.op("dve", [g.tpb[7], t_c], [t_c], lambda: V.reciprocal(out=r2[:, N], in_=g.pb[7][:, N]))
                    k.op("dve", [t_c, t_lam], [t_c], lambda: V.tensor_scalar(out=r2[:, N], in0=r2[:, N], scalar1=lam, scalar2=None, op0=ALU.mult))
                    for s_ in range(2):
                        k.op("dve", [g.tpb[2 + s_], t_c], [t_c], lambda s_=s_: V.tensor_tensor(out=A[s_][:, N], in0=g.pb[2 + s_][:, N], in1=r1[:, N], op=ALU.mult))
                        k.op("dve", [g.tpb[4 + s_], t_c], [t_c], lambda s_=s_: V.tensor_tensor(out=B[:, N], in0=g.pb[4 + s_][:, N], in1=r2[:, N], op=ALU.mult))
                        k.op("dve", [t_c], [t_c], lambda s_=s_: V.tensor_tensor(out=A[s_][:, N], in0=A[s_][:, N], in1=B[:, N], op=ALU.subtract))
                        k.op("act", [t_c], [t_c], lambda s_=s_: nc.scalar.activation(out=sq[s_][:, N], in_=A[s_][:, N], func=AF.Square))
                    pq, tpq = g.pb[6], g.tpb[6]
                    for s_ in range(2):
                        k.op("pe", [t_c, g.t_const], [tpq], lambda s_=s_: nc.tensor.matmul(pq[:, N], g.onesdiv_b[:, :], sq[s_][:, N], start=(s_ == 0), stop=(s_ == 1)))
                    k.op("dve", [tpq, t_c], [t_c], lambda: V.tensor_scalar(out=rr[:, N], in0=pq[:, N], scalar1=0.5, scalar2=RMS_EPS, op0=ALU.mult, op1=ALU.add))
                    k.op("act", [t_c], [t_c], lambda: nc.scalar.activation(out=rr[:, N], in_=rr[:, N], func=AF.Sqrt))
                    k.op("dve", [t_c], [t_c], lambda: V.reciprocal(out=rr[:, N], in_=rr[:, N]))
                    for s_ in range(2):
                        k.op("dve", [t_c, t_lam], [t_oT], lambda s_=s_: V.scalar_tensor_tensor(out=oT[:, 2 * h + s_, c0:c0 + n], in0=A[s_][:, N], scalar=gsc[:, s_:s_ + 1], in1=rr[:, N], op0=ALU.mult, op1=ALU.mult), part=True)
        emit_post(k, g, m, L, oT, t_oT, wts.do[0], wts.do[1], lambda pn: list(range(NCH)), lambda pn, ci: ci * 128, 2, x_cur, t_x)


KIND = {0: "gqa", 1: "pool", 2: "diff", 3: "gqa"}
JIDX = {0: 0, 1: 0, 2: 0, 3: 1}


def build_program(layers=(0, 1, 2, 3), stop_after=None):
    k = KB()
    nc = k.nc
    g = W()
    g.layers = list(layers)
    ext = lambda name, shape: k.dram(name, shape, F32, "ExternalInput")
    g.d_xin = ext("xin", [T, D])
    g.d_cc = ext("cc", [2, D])
    NL = len(g.layers)
    g.li = {L: i for i, L in enumerate(g.layers)}
    g.d_adaw = ext("adaw", [NL, D, MCOLS])
    g.d_adab = ext("adab", [NL, 1, MCOLS])
    g.d_ln_g = ext("ln_g", [DEPTH, 2, D])
    g.d_ln_b = ext("ln_b", [DEPTH, 2, D])
    g.d_ident = ext("ident", [128, 128])
    g.d_rotT = ext("rotT", [128, 128])
    g.d_sel = ext("sel", [16, NEXP * 128])
    g.d_wr = ext("wr", [DEPTH, D, 20])
    g.d_rb = ext("rb", [DEPTH, 1, 20])
    g.d_w1 = ext("w1s", [NL, 2 * D, DEXP])
    g.d_w3 = ext("w3s", [NL, 2 * D, DEXP])
    g.d_w2 = ext("w2s", [NL, 2 * DEXP, D])
    kinds = set(KIND[L] for L in layers)
    if "gqa" in kinds or "diff" in kinds:
        g.d_cosT = ext("cosT", [128, TX])
        g.d_sinT = ext("sinT", [128, TX])
    if "gqa" in kinds:
        g.d_wq = ext("wq", [2, 512, D])
        g.d_wk = ext("wk", [2, 512, 1024])
        g.d_wv = ext("wv", [2, 512, 1024])
        g.d_wo = ext("wo", [2, 512, D])
        g.d_qn_g = ext("qn_g", [2, 128])
        g.d_kn_g = ext("kn_g", [2, 128])
    if "pool" in kinds:
        g.d_poolw = ext("poolw", [512, 1024])
        g.d_pool_b = ext("pool_b", [1, D])
        g.d_pool_scale = ext("pool_scale", [1, D])
        g.d_hsel = ext("hsel", [256, 32])
        g.d_hmask = ext("hmask", [1, 32])
        g.d_pinv = ext("pinv", [1, 4 * T])
    if "diff" in kinds:
        g.d_dq = ext("dq", [512, D])
        g.d_dk = ext("dk", [512, D])
        g.d_dv = ext("dv", [512, D])
        g.d_do = ext("do", [512, D])
        g.d_dlam = ext("dlam", [4, 128])
        g.d_subln = ext("subln", [1, 256])
    g.d_xout = k.dram("xout", [T, D], F32, "ExternalOutput")
    g.mods_part = k.dram("mods_part", [8, MCOLS], F32)
    g.mods_all = k.dram("mods_all", [64, MCOLS], F32)
    g.t_mods_part, g.t_mods_all = Trk("mods_part"), Trk("mods_all")
    g.u = k.dram("u", [T, D], F32)
    g.t_u = Trk("u")
    g.xa = k.dram("xa", [T, D], F32)
    g.xb = k.dram("xb", [T, D], F32)
    g.t_xa, g.t_xb = Trk("xa"), Trk("xb")
    g.hid_d = k.dram("hid_d", [48, 128, T], BF16)
    g.t_hid = Trk("hid_d")
    g.wts = {}

    def prefetch(L):
        if L in g.wts or L not in g.layers:
            return
        w = W()
        kind, j = KIND[L], JIDX[L]
        if kind == "gqa":
            w.wk = gather_weight(k, g, "wk%d" % L, g.d_wk[j], 512, 1024)
            w.wv = gather_weight(k, g, "wv%d" % L, g.d_wv[j], 512, 1024)
            w.wq = gather_weight(k, g, "wq%d" % L, g.d_wq[j], 512, D)
            w.wo = gather_weight(k, g, "wo%d" % L, g.d_wo[j], 512, D)
        elif kind == "pool":
            w.pw = gather_weight(k, g, "pw%d" % L, g.d_poolw, 512, 1024)
        else:
            w.dk = gather_weight(k, g, "dk%d" % L, g.d_dk, 512, D)
            w.dv = gather_weight(k, g, "dv%d" % L, g.d_dv, 512, D)
            w.dq = gather_weight(k, g, "dq%d" % L, g.d_dq, 512, D)
            w.do = gather_weight(k, g, "do%d" % L, g.d_do, 512, D)
        w.w1 = gather_weight(k, g, "w1_%d" % L, g.d_w1[g.li[L]], 2 * D, DEXP)
        w.w3 = gather_weight(k, g, "w3_%d" % L, g.d_w3[g.li[L]], 2 * D, DEXP)
        w.w2 = gather_weight(k, g, "w2_%d" % L, g.d_w2[g.li[L]], 2 * DEXP, D)
        g.wts[L] = w

    def after_exchange(L):
        i = g.layers.index(L)
        if i + 2 < len(g.layers):
            prefetch(g.layers[i + 2])
    g.after_exchange = after_exchange

    setup_common(k, g)
    emit_mods(k, g)
    prefetch(g.layers[0])
    if len(g.layers) > 1:
        prefetch(g.layers[1])
    if "gqa" in kinds or "diff" in kinds:
        g.cosT = k.sb("cosT", [128, TX], F32)
        g.sinT = k.sb("sinT", [128, TX], F32)
        g.t_rope = Trk("rope")
        k.dma("sp", g.cosT[:, :], g.d_cosT[:, :], [], [g.t_rope], g.t_rope)
        k.dma("sp", g.sinT[:, :], g.d_sinT[:, :], [], [g.t_rope], g.t_rope)

    x_cur, t_x = g.d_xin, Trk("xin")
    xbufs = [(g.xa, g.t_xa), (g.xb, g.t_xb)]
    xi = 0
    pending_ln = None
    for L in g.layers:
        with k.scope():
            m = W()
            load_layer_mods(k, g, L, m)
            if pending_ln is not None:
                dst, t_dst = xbufs[xi % 2]
                xi += 1
                emit_front(k, g, m, 0, g.u, g.t_u, None, None, ln=pending_ln, dst=dst, t_dst=t_dst, do_T=False)
                x_cur, t_x = dst, t_dst
                pending_ln = None
            kind, j = KIND[L], JIDX[L]
            try:
                if kind == "gqa":
                    emit_gqa_layer(k, g, m, L, j, x_cur, t_x)
                elif kind == "pool":
                    emit_pool_layer(k, g, m, L, x_cur, t_x)
                else:
                    emit_diff_layer(k, g, m, L, x_cur, t_x)
            except StopEmit:
                pending_ln = (L, 0)
                break
            if stop_after == "mixer":
                pending_ln = (L, 0)
                break
            dst, t_dst = xbufs[xi % 2]
            xi += 1
            with k.scope():
                hT = k.sb("hT2", [128, NCH, T], BF16)
                t_hT = Trk("hT2")
                emit_front(k, g, m, 1, g.u, g.t_u, hT, t_hT, ln=(L, 0), dst=dst, t_dst=t_dst)
                x_cur, t_x = dst, t_dst
                emit_moe_up(k, g, m, L, hT, t_hT)
            emit_moe_down(k, g, m, L, x_cur, t_x)
            pending_ln = (L, 1)
    with k.scope():
        m = W()
        t_out = Trk("xout")
        emit_front(k, g, m, 0, g.u, g.t_u, None, None, ln=pending_ln, dst=g.d_xout, t_dst=t_out, do_T=False)
    k.barrier()
    return k.nc


def _consts():
    ident = np.eye(128, dtype=np.float32)
    rm = np.zeros((128, 128), np.float32)
    for a in range(2):
        for f in range(32):
            rm[a * 64 + f, a * 64 + 32 + f] = -1.0
            rm[a * 64 + 32 + f, a * 64 + f] = 1.0
    rotT = np.ascontiguousarray(rm.T)
    sel = np.zeros((16, NEXP * 128), np.float32)
    for e in range(NEXP):
        sel[e, e * 128:(e + 1) * 128] = 1.0
    return ident, rotT, sel


def _rope_tables(core):
    t = np.arange(core * TX, (core + 1) * TX)
    row = (t // GRID_W).astype(np.float32)
    col = (t % GRID_W).astype(np.float32)
    inv_freq = (np.float32(10000.0) ** (-np.arange(32, dtype=np.float32) / np.float32(32))).astype(np.float32)
    ang = np.stack([row[:, None] * inv_freq, col[:, None] * inv_freq], axis=1)
    cos = np.cos(ang).astype(np.float32)
    sin = np.sin(ang).astype(np.float32)
    cosT = np.ascontiguousarray(np.broadcast_to(cos[:, :, None, :], (TX, 2, 2, 32)).reshape(TX, 128).T)
    sinT = np.ascontiguousarray(np.broadcast_to(sin[:, :, None, :], (TX, 2, 2, 32)).reshape(TX, 128).T)
    return cosT, sinT


def _pool_tables(core):
    pinv = np.zeros((4, T), np.float32)
    for gi, w in enumerate(POOL_WINDOWS):
        for (n, t0, o0, cnt) in ((SEQ, core * TX, 0, TX), (CTX, core * TC, TX, TC)):
            t = np.arange(t0, t0 + cnt)
            lo = np.clip(t - w // 2, 0, n - 1)
            hi = np.clip(t + w // 2 - 1, 0, n - 1)
            pinv[gi, o0:o0 + cnt] = 1.0 / (hi - lo + 1).astype(np.float32)
    hsel = np.zeros((256, 32), np.float32)
    hmask = np.zeros((1, 32), np.float32)
    if core > 0:
        for i in range(8):
            hsel[(core - 1) * 32 + 8 + i, i] = 1.0
            hsel[(core - 1) * 32 + 24 + i, 16 + i] = 1.0
        hmask[0, 0:8] = 1.0
        hmask[0, 16:24] = 1.0
    if core < NCORES - 1:
        for i in range(8):
            hsel[(core + 1) * 32 + i, 8 + i] = 1.0
            hsel[(core + 1) * 32 + 16 + i, 24 + i] = 1.0
        hmask[0, 8:16] = 1.0
        hmask[0, 24:32] = 1.0
    return pinv.reshape(1, 4 * T), hsel, hmask


def make_in_maps(inp, layers, x_rows=None):
    ident, rotT, sel = _consts()
    kinds = set(KIND[L] for L in layers)
    f32 = lambda a: np.ascontiguousarray(a, dtype=np.float32)
    maps = []
    cc = f32(np.stack([inp["c"][0], inp["c_ctx"]]))
    wr = f32(np.concatenate([inp["moe_rg_w"], inp["moe_re_w"]], axis=2))
    rb = f32(np.concatenate([inp["moe_rg_b"], inp["moe_re_b"]], axis=1))[:, None, :]
    ls = list(layers)
    NL = len(ls)
    adaw6 = inp["ada_w"][ls].reshape(NL, D, 6, NCORES, 512)
    adab6 = inp["ada_b"][ls].reshape(NL, 6, NCORES, 512)
    w1 = inp["moe_w1"][ls].reshape(NL, NEXP * D, DEXP)
    w3 = inp["moe_w3"][ls].reshape(NL, NEXP * D, DEXP)
    w2 = inp["moe_w2"][ls].reshape(NL, NEXP * DEXP, D)
    for c in range(NCORES):
        mp = {}
        if x_rows is not None:
            mp["xin"] = f32(x_rows[c])
        else:
            mp["xin"] = f32(np.concatenate([inp["x"][0, c * TX:(c + 1) * TX], inp["ctx"][0, c * TC:(c + 1) * TC]], axis=0))
        mp["cc"] = cc
        mp["adaw"] = f32(adaw6[:, :, :, c, :].reshape(NL, D, MCOLS))
        mp["adab"] = f32(adab6[:, :, c, :].reshape(NL, 1, MCOLS))
        mp["ln_g"] = f32(inp["ln_g"])
        mp["ln_b"] = f32(inp["ln_b"])
        mp["ident"], mp["rotT"], mp["sel"] = ident, rotT, sel
        mp["wr"], mp["rb"] = wr, rb
        mp["w1s"] = f32(w1[:, c * 2 * D:(c + 1) * 2 * D])
        mp["w3s"] = f32(w3[:, c * 2 * D:(c + 1) * 2 * D])
        mp["w2s"] = f32(w2[:, c * 2 * DEXP:(c + 1) * 2 * DEXP])
        if "gqa" in kinds or "diff" in kinds:
            mp["cosT"], mp["sinT"] = _rope_tables(c)
        if "gqa" in kinds:
            rs = slice(c * 512, (c + 1) * 512)
            mp["wq"] = f32(inp["attn_wq"][:, rs])
            mp["wk"] = f32(inp["attn_wk"][:, rs])
            mp["wv"] = f32(inp["attn_wv"][:, rs])
            mp["wo"] = f32(inp["attn_wo"][:, rs])
            mp["qn_g"] = f32(inp["attn_qn_g"])
            mp["kn_g"] = f32(inp["attn_kn_g"])
        if "pool" in kinds:
            mp["poolw"] = f32(inp["pool_w"][0].reshape(4 * 1024, 1024)[c * 512:(c + 1) * 512])
            mp["pool_b"] = f32(inp["pool_b"][0].reshape(1, D))
            mp["pool_scale"] = f32(inp["pool_scale"][0].reshape(1, D))
            mp["pinv"], mp["hsel"], mp["hmask"] = _pool_tables(c)
        if "diff" in kinds:
            rs = slice(c * 512, (c + 1) * 512)
            mp["dq"] = f32(inp["diff_wq"][0, rs])
            mp["dk"] = f32(inp["diff_wk"][0, rs])
            mp["dv"] = f32(inp["diff_wv"][0, rs])
            mp["do"] = f32(inp["diff_wo"][0, rs])
            mp["dlam"] = f32(np.concatenate([inp["diff_lq1"], inp["diff_lk1"], inp["diff_lq2"], inp["diff_lk2"]], axis=0))
            mp["subln"] = f32(inp["diff_subln_g"])
        maps.append(mp)
    return maps


_PROGS = {}


def run_layers(inp, layers, x_rows=None, stop_after=None):
    key = (tuple(layers), stop_after, os.environ.get('DBG_STOP'), os.environ.get('DBG_NOQD'), os.environ.get('DBG_Q1'))
    if key not in _PROGS:
        _PROGS[key] = build_program(layers, stop_after)
    nc = _PROGS[key]
    maps = make_in_maps(inp, layers, x_rows)
    res = run_bass_kernel_spmd(nc, maps, core_ids=list(range(NCORES)))
    return [r["xout"] for r in res.results]


def kernel(**inp):
    rows = run_layers(inp, (0, 1, 2, 3))
    out = np.concatenate([r[0:TX] for r in rows], axis=0)[None]
    return np.ascontiguousarray(out, dtype=np.float32)
```
